# Optimizing a Trainium2 kernel written in Bass

```python
import jax, jax.numpy as jnp
from jax import lax
import numpy as np

D_MODEL = 2048
BATCH = 2
SEQ = 4096
DEPTH = 2

GRID_W = 64
CTX_LEN = 256
HEAD_DIM = 128
SWA_HEADS = 4
SWA_KV_HEADS = 2
SWA_WINDOW = 128
SWA_BLOCK = 128
RET_HEADS = 4
RET_DK = 128
RET_DV = 128
RET_CHUNK = 128
MLA_HEADS = 4
MLA_Q_RANK = 384
MLA_KV_RANK = 128
MLA_NOPE = 128
MLA_ROPE = 64
MLA_V = 128
MLA_QBLOCK = 128
HGRN_HEADS = 4
HGRN_DK = 128
HGRN_DV = 128
HGRN_CHUNK = 16
MIX_WIDTH = SWA_HEADS * HEAD_DIM + RET_HEADS * RET_DV + MLA_HEADS * MLA_V + HGRN_HEADS * HGRN_DV
IN_WIDTHS = (SWA_HEADS * HEAD_DIM, SWA_KV_HEADS * HEAD_DIM, SWA_KV_HEADS * HEAD_DIM,
             RET_HEADS * RET_DK, RET_HEADS * RET_DK, RET_HEADS * RET_DV, RET_HEADS * RET_DV,
             MLA_Q_RANK, MLA_KV_RANK, MLA_ROPE,
             HGRN_HEADS * HGRN_DK, HGRN_HEADS * HGRN_DK, HGRN_HEADS * HGRN_DK, HGRN_HEADS * HGRN_DV, HGRN_HEADS * HGRN_DV)
IN_COLS = sum(IN_WIDTHS)
N_GROUPS = 4
EXPERTS_PER_GROUP = 8
N_EXPERTS = N_GROUPS * EXPERTS_PER_GROUP
TOP_K = 2
EXPERT_FF = 512
MOE_BLOCK = 128
ROPE_BASE = 10000.0
EPS = 1e-6
DEEPNORM_ALPHA = (2 * DEPTH) ** 0.25
DEEPNORM_BETA = (8 * DEPTH) ** -0.25

kernel_name = "hybrid_parallel_heads_dit_moe"

F32 = jnp.float32


def _layer_norm(x, g=None, b=None):
    xf = x.astype(F32)
    xc = xf - jnp.mean(xf, -1, keepdims=True)
    y = xc * lax.rsqrt(jnp.mean(xc * xc, -1, keepdims=True) + EPS)
    if g is not None:
        y = y * g.astype(F32) + b.astype(F32)
    return y.astype(x.dtype)


def _rms_norm(x, g):
    xf = x.astype(F32)
    y = xf * lax.rsqrt(jnp.mean(xf * xf, -1, keepdims=True) + EPS)
    return (y * g.astype(F32)).astype(x.dtype)


def _modulate(x, shift, scale):
    return _layer_norm(x) * (1.0 + scale) + shift


def _heads(a, h):
    return a.reshape(a.shape[:-1] + (h, a.shape[-1] // h))


def _bhtd(a, h):
    return jnp.swapaxes(_heads(a, h), 1, 2)


def _axial_angles(row, col, rot_dim):
    n_freq = rot_dim // 4
    inv = ROPE_BASE ** (-jnp.arange(n_freq, dtype=F32) / n_freq)
    return jnp.concatenate([row.astype(F32)[:, None] * inv, col.astype(F32)[:, None] * inv], -1)


def _rope(x, ang):
    half = x.shape[-1] // 2
    x1, x2 = x[..., :half], x[..., half:]
    cos, sin = jnp.cos(ang).astype(x.dtype), jnp.sin(ang).astype(x.dtype)
    return jnp.concatenate([x1 * cos - x2 * sin, x2 * cos + x1 * sin], -1)


def _swa(q, k, v, qc, kc, vc, sink, need_ctx):
    B, T, _, dh = q.shape
    L = kc.shape[1]
    nb = T // SWA_BLOCK
    g = SWA_HEADS // SWA_KV_HEADS
    scale = dh ** -0.5
    qb = q.reshape(B, nb, SWA_BLOCK, SWA_KV_HEADS, g, dh)
    pad = ((0, 0), (SWA_BLOCK, SWA_BLOCK), (0, 0), (0, 0))
    kb = jnp.pad(k, pad).reshape(B, nb + 2, SWA_BLOCK, SWA_KV_HEADS, dh)
    vb = jnp.pad(v, pad).reshape(B, nb + 2, SWA_BLOCK, SWA_KV_HEADS, dh)
    band = lambda a: jnp.concatenate([a[:, :-2], a[:, 1:-1], a[:, 2:]], axis=2)
    kband, vband = band(kb), band(vb)
    s_loc = jnp.einsum('bnqhgd,bnkhd->bnhgqk', qb, kband).astype(F32) * scale
    qi = jnp.arange(SWA_BLOCK)[:, None]
    ki = jnp.arange(3 * SWA_BLOCK)[None, :] - SWA_BLOCK
    kabs = jnp.arange(nb)[:, None, None] * SWA_BLOCK + ki[None]
    mask = (jnp.abs(ki - qi) <= SWA_WINDOW)[None] & (kabs >= 0) & (kabs < T)
    s_loc = jnp.where(mask[None, :, None, None], s_loc, -jnp.inf)
    s_ctx = jnp.einsum('bnqhgd,blhd->bnhgql', qb, kc).astype(F32) * scale
    sink_f = sink.astype(F32).reshape(1, 1, SWA_KV_HEADS, g, 1, 1)
    s_sink = jnp.broadcast_to(sink_f, s_loc.shape[:-1] + (1,))
    p = jax.nn.softmax(jnp.concatenate([s_loc, s_ctx, s_sink], -1), axis=-1)
    nk = 3 * SWA_BLOCK
    o = (jnp.einsum('bnhgqk,bnkhd->bnqhgd', p[..., :nk].astype(v.dtype), vband)
         + jnp.einsum('bnhgql,blhd->bnqhgd', p[..., nk:nk + L].astype(v.dtype), vc))
    o_lat = o.reshape(B, T, SWA_HEADS * dh)
    o_ctx = None
    if need_ctx:
        qcg = qc.reshape(B, L, SWA_KV_HEADS, g, dh)
        sc = jnp.einsum('blhgd,bmhd->bhglm', qcg, kc).astype(F32) * scale
        sc_sink = jnp.broadcast_to(sink.astype(F32).reshape(1, SWA_KV_HEADS, g, 1, 1), sc.shape[:-1] + (1,))
        pc = jax.nn.softmax(jnp.concatenate([sc, sc_sink], -1), axis=-1)
        o_ctx = jnp.einsum('bhglm,bmhd->blhgd', pc[..., :L].astype(vc.dtype), vc).reshape(B, L, SWA_HEADS * dh)
    return o_lat, o_ctx


def _chunk_scan(q, k, v, g, s0, chunk, per_dim):
    dt = v.dtype
    q, k, v, g, s0 = (a.astype(F32) for a in (q, k, v, g, s0))
    B, H, T, _ = q.shape
    nc = T // chunk
    rs = lambda a: a.reshape(B, H, nc, chunk, a.shape[-1])
    q, k, v, g = rs(q), rs(k), rs(v), rs(g)
    b = jnp.cumsum(g, axis=3)
    b_last = b[:, :, :, -1:, :]
    causal = jnp.tril(jnp.ones((chunk, chunk), bool))
    if per_dim:
        diff = b[:, :, :, :, None, :] - b[:, :, :, None, :, :]
        dec = jnp.exp(jnp.where(causal[:, :, None], diff, -jnp.inf))
        scores = jnp.einsum('bhcnmd,bhcmd->bhcnm', q[:, :, :, :, None, :] * dec, k)
    else:
        bs = b[..., 0]
        diff = bs[..., :, None] - bs[..., None, :]
        dec = jnp.exp(jnp.where(causal, diff, -jnp.inf))
        scores = jnp.einsum('bhcnd,bhcmd->bhcnm', q, k) * dec
    o_intra = jnp.einsum('bhcnm,bhcmv->bhcnv', scores, v)
    q_in = q * jnp.exp(b)
    k_st = k * jnp.exp(b_last - b)
    a_last = jnp.exp(b_last[:, :, :, 0, :])

    def step(S, xs):
        qi, ki, vi, ai = xs
        o = jnp.einsum('bhnd,bhdv->bhnv', qi, S)
        S = ai[..., :, None] * S + jnp.einsum('bhmd,bhmv->bhdv', ki, vi)
        return S, o

    xs = tuple(jnp.moveaxis(a, 2, 0) for a in (q_in, k_st, v, a_last))
    s_T, o_inter = lax.scan(step, s0, xs)
    o = o_intra + jnp.moveaxis(o_inter, 0, 2)
    return o.reshape(B, H, T, -1).astype(dt), s_T


def _bidir_scan(q, v, k_dirs, g_dirs, qc, vc, kc_dirs, gc_dirs, chunk, per_dim):
    B, H, _, dk = q.shape
    s0 = jnp.zeros((B, H, dk, v.shape[-1]), F32)
    ident = lambda a: a
    flip = lambda a: jnp.flip(a, axis=2)
    o_lat, o_ctx = [], []
    for d, tf in enumerate((ident, flip)):
        oc, sc = _chunk_scan(tf(qc), tf(kc_dirs[d]), tf(vc), tf(gc_dirs[d]), s0, chunk, per_dim)
        o, _ = _chunk_scan(tf(q), tf(k_dirs[d]), tf(v), tf(g_dirs[d]), sc, chunk, per_dim)
        o_lat.append(tf(o))
        o_ctx.append(tf(oc))
    return o_lat[0] + o_lat[1], o_ctx[0] + o_ctx[1]


def _mla(qn, qr, kn, kr, v, qnc, qrc, knc, krc, vc, need_ctx):
    B, T, H, _ = qn.shape
    L = knc.shape[1]
    nb = T // MLA_QBLOCK
    scale = (MLA_NOPE + MLA_ROPE) ** -0.5
    k_n = jnp.concatenate([knc, kn], 1)
    k_r = jnp.concatenate([krc, kr], 1)
    v_all = jnp.concatenate([vc, v], 1)

    def block(args):
        qbn, qbr = args
        s = (jnp.einsum('bqhd,bkhd->bhqk', qbn, k_n) + jnp.einsum('bqhr,bkr->bhqk', qbr, k_r)).astype(F32) * scale
        p = jax.nn.softmax(s, axis=-1).astype(v_all.dtype)
        return jnp.einsum('bhqk,bkhd->bqhd', p, v_all)

    to_blocks = lambda a: jnp.moveaxis(a.reshape((B, nb, MLA_QBLOCK) + a.shape[2:]), 1, 0)
    o = lax.map(block, (to_blocks(qn), to_blocks(qr)))
    o_lat = jnp.moveaxis(o, 0, 1).reshape(B, T, H * MLA_V)
    o_ctx = None
    if need_ctx:
        s = (jnp.einsum('blhd,bmhd->bhlm', qnc, knc) + jnp.einsum('blhr,bmr->bhlm', qrc, krc)).astype(F32) * scale
        pc = jax.nn.softmax(s, axis=-1).astype(vc.dtype)
        o_ctx = jnp.einsum('bhlm,bmhd->blhd', pc, vc).reshape(B, L, H * MLA_V)
    return o_lat, o_ctx


def _ret_out(o, gate, gn):
    y = _layer_norm(jnp.swapaxes(o, 1, 2))
    y = y.reshape(y.shape[:2] + (-1,)) * gn
    return jax.nn.silu(gate) * y


def _hgrn_out(o, gate, gn):
    y = _rms_norm(jnp.swapaxes(o, 1, 2), gn.reshape(HGRN_HEADS, HGRN_DV))
    return jax.nn.silu(gate) * y.reshape(y.shape[:2] + (-1,))


def _token_mixers(h_lat, h_ctx, w_in, w_out, swa_sink, ret_s, ret_gn, q_norm, kv_norm, w_uq, w_ukv,
                  lb, hgrn_gn, ang_swa, ang_mla, need_ctx):
    B, T, _ = h_lat.shape
    L = h_ctx.shape[1]
    cuts = np.cumsum(IN_WIDTHS)[:-1].tolist()
    (sq, sk, sv, rq, rk, rv, rg, cq, ckv, kr, hq, hff, hfb, hi, hg) = jnp.split(h_lat @ w_in, cuts, axis=-1)
    (sqc, skc, svc, rqc, rkc, rvc, rgc, cqc, ckvc, krc, hqc, hffc, hfbc, hic, hgc) = jnp.split(h_ctx @ w_in, cuts, axis=-1)

    a_lat, a_ctx = _swa(_rope(_heads(sq, SWA_HEADS), ang_swa[:, None]), _rope(_heads(sk, SWA_KV_HEADS), ang_swa[:, None]),
                        _heads(sv, SWA_KV_HEADS), _heads(sqc, SWA_HEADS), _heads(skc, SWA_KV_HEADS),
                        _heads(svc, SWA_KV_HEADS), swa_sink, need_ctx)

    log_gamma = jnp.log1p(-jnp.exp2(-ret_s.astype(F32)))
    ret_g = lambda n: [jnp.broadcast_to(log_gamma[d][None, :, None, None], (B, RET_HEADS, n, 1)) for d in range(2)]
    ksc = RET_DK ** -0.5
    k_r, kc_r = _bhtd(rk, RET_HEADS) * ksc, _bhtd(rkc, RET_HEADS) * ksc
    b_lat, b_ctx = _bidir_scan(_bhtd(rq, RET_HEADS), _bhtd(rv, RET_HEADS), [k_r, k_r], ret_g(T),
                               _bhtd(rqc, RET_HEADS), _bhtd(rvc, RET_HEADS), [kc_r, kc_r], ret_g(L), RET_CHUNK, False)

    def mla_proj(cq_, ckv_):
        qq = _heads(_rms_norm(cq_, q_norm) @ w_uq, MLA_HEADS)
        kv = _heads(_rms_norm(ckv_, kv_norm) @ w_ukv, MLA_HEADS)
        return qq[..., :MLA_NOPE], qq[..., MLA_NOPE:], kv[..., :MLA_NOPE], kv[..., MLA_NOPE:]
    qn, qr, kn, vv = mla_proj(cq, ckv)
    qnc, qrc, knc, vvc = mla_proj(cqc, ckvc)
    c_lat, c_ctx = _mla(qn, _rope(qr, ang_mla[:, None]), kn, _rope(kr, ang_mla), vv,
                        qnc, qrc, knc, krc, vvc, need_ctx)

    def hgrn_dirs(zf, zb):
        ks_, gs_ = [], []
        for d, z in enumerate((zf, zb)):
            lbd = lb[d].reshape(HGRN_HEADS, 1, HGRN_DK)
            f = lbd + (1.0 - lbd) * jax.nn.sigmoid(_bhtd(z, HGRN_HEADS).astype(F32))
            ks_.append(1.0 - f)
            gs_.append(jnp.log(f))
        return ks_, gs_
    hk, hgd = hgrn_dirs(hff, hfb)
    hkc, hgdc = hgrn_dirs(hffc, hfbc)
    d_lat, d_ctx = _bidir_scan(jax.nn.silu(_bhtd(hq, HGRN_HEADS)), _bhtd(hi, HGRN_HEADS), hk, hgd,
                               jax.nn.silu(_bhtd(hqc, HGRN_HEADS)), _bhtd(hic, HGRN_HEADS), hkc, hgdc, HGRN_CHUNK, True)

    o_lat = jnp.concatenate([a_lat, _ret_out(b_lat, rg, ret_gn), c_lat, _hgrn_out(d_lat, hg, hgrn_gn)], -1) @ w_out
    o_ctx = None
    if need_ctx:
        o_ctx = jnp.concatenate([a_ctx, _ret_out(b_ctx, rgc, ret_gn), c_ctx, _hgrn_out(d_ctx, hgc, hgrn_gn)], -1) @ w_out
    return o_lat, o_ctx


def _hier_moe(h, w_rg, w_re, w1, w3, w2):
    n, d = h.shape
    pg = jax.nn.softmax((h @ w_rg).astype(F32), axis=-1)
    grp = jnp.argmax(pg, axis=-1)
    p_grp = jnp.take_along_axis(pg, grp[:, None], axis=-1)
    le = (h @ w_re).astype(F32).reshape(n, N_GROUPS, EXPERTS_PER_GROUP)
    le = jnp.take_along_axis(le, grp[:, None, None], axis=1)[:, 0]
    top_p, top_i = lax.top_k(jax.nn.softmax(le, axis=-1), TOP_K)
    gate = p_grp * top_p / jnp.sum(top_p, -1, keepdims=True)
    eid = (grp[:, None] * EXPERTS_PER_GROUP + top_i).reshape(-1).astype(jnp.int32)
    tok = jnp.repeat(jnp.arange(n, dtype=jnp.int32), TOP_K)
    wgt = gate.reshape(-1)
    order = jnp.argsort(eid)
    eid, tok, wgt = eid[order], tok[order], wgt[order]
    counts = jnp.zeros((N_EXPERTS,), jnp.int32).at[eid].add(1)
    padded = (counts + MOE_BLOCK - 1) // MOE_BLOCK * MOE_BLOCK
    start = jnp.cumsum(counts) - counts
    pend = jnp.cumsum(padded)
    pstart = pend - padded
    dest = pstart[eid] + jnp.arange(eid.shape[0], dtype=jnp.int32) - start[eid]
    n_blocks = (n * TOP_K + N_EXPERTS * (MOE_BLOCK - 1) + MOE_BLOCK - 1) // MOE_BLOCK
    xbuf = jnp.zeros((n_blocks * MOE_BLOCK, d), h.dtype).at[dest].set(h[tok])
    blk_e = jnp.minimum(jnp.searchsorted(pend, jnp.arange(n_blocks, dtype=jnp.int32) * MOE_BLOCK, side='right'),
                        N_EXPERTS - 1)

    def expert_block(args):
        xb, e = args
        return (jax.nn.silu(xb @ w1[e]) * (xb @ w3[e])) @ w2[e]

    ybuf = lax.map(expert_block, (xbuf.reshape(n_blocks, MOE_BLOCK, d), blk_e)).reshape(-1, d)
    y = jax.ops.segment_sum(ybuf[dest].astype(F32) * wgt[:, None], tok, num_segments=n)
    return y.astype(h.dtype)


def setup_inputs(seed: int = 0) -> dict:
    key = jax.random.key(seed)
    ks = jax.random.split(key, 28)
    nrm = lambda k, shape, s: jax.random.normal(k, shape, F32) * s
    D = D_MODEL
    return {
        "x": nrm(ks[0], (BATCH, SEQ, D), 1.0),
        "c": nrm(ks[1], (BATCH, D), 1.0),
        "ctx": nrm(ks[2], (BATCH, CTX_LEN, D), 1.0),
        "c_ctx": nrm(ks[3], (D,), 1.0),
        "w_ada": nrm(ks[4], (DEPTH, D, 6 * D), 0.5 * D ** -0.5),
        "b_ada": nrm(ks[5], (DEPTH, 6 * D), 0.02),
        "w_in": nrm(ks[6], (DEPTH, D, IN_COLS), D ** -0.5),
        "swa_sink": nrm(ks[7], (DEPTH, SWA_HEADS), 1.0),
        "ret_decay_exp": 5.0 + jnp.arange(RET_HEADS, dtype=F32) + nrm(ks[8], (DEPTH, 2, RET_HEADS), 0.2),
        "ret_gn": 1.0 + nrm(ks[9], (DEPTH, RET_HEADS * RET_DV), 0.1),
        "mla_q_norm": 1.0 + nrm(ks[10], (DEPTH, MLA_Q_RANK), 0.1),
        "mla_kv_norm": 1.0 + nrm(ks[11], (DEPTH, MLA_KV_RANK), 0.1),
        "mla_w_uq": nrm(ks[12], (DEPTH, MLA_Q_RANK, MLA_HEADS * (MLA_NOPE + MLA_ROPE)), MLA_Q_RANK ** -0.5),
        "mla_w_ukv": nrm(ks[13], (DEPTH, MLA_KV_RANK, MLA_HEADS * (MLA_NOPE + MLA_V)), MLA_KV_RANK ** -0.5),
        "hgrn_lb_logits": nrm(ks[14], (DEPTH, 2, HGRN_HEADS * HGRN_DK), 0.5),
        "hgrn_gn": 1.0 + nrm(ks[15], (DEPTH, HGRN_HEADS * HGRN_DV), 0.1),
        "w_out": nrm(ks[16], (DEPTH, MIX_WIDTH, D), DEEPNORM_BETA * MIX_WIDTH ** -0.5),
        "ln1_g": 1.0 + nrm(ks[17], (DEPTH, D), 0.1),
        "ln1_b": nrm(ks[18], (DEPTH, D), 0.02),
        "router_group": nrm(ks[19], (DEPTH, D, N_GROUPS), D ** -0.5),
        "router_expert": nrm(ks[20], (DEPTH, D, N_EXPERTS), D ** -0.5),
        "moe_w1": nrm(ks[21], (DEPTH, N_EXPERTS, D, EXPERT_FF), D ** -0.5),
        "moe_w3": nrm(ks[22], (DEPTH, N_EXPERTS, D, EXPERT_FF), D ** -0.5),
        "moe_w2": nrm(ks[23], (DEPTH, N_EXPERTS, EXPERT_FF, D), DEEPNORM_BETA * EXPERT_FF ** -0.5),
        "ln2_g": 1.0 + nrm(ks[24], (DEPTH, D), 0.1),
        "ln2_b": nrm(ks[25], (DEPTH, D), 0.02),
    }


def reference(x, c, ctx, c_ctx, w_ada, b_ada, w_in, swa_sink, ret_decay_exp, ret_gn, mla_q_norm, mla_kv_norm,
              mla_w_uq, mla_w_ukv, hgrn_lb_logits, hgrn_gn, w_out, ln1_g, ln1_b, router_group, router_expert,
              moe_w1, moe_w3, moe_w2, ln2_g, ln2_b):
    B, T, D = x.shape
    L = ctx.shape[1]
    rows = T // GRID_W
    row = jnp.repeat(jnp.arange(rows, dtype=jnp.int32), GRID_W)
    col = jnp.tile(jnp.arange(GRID_W, dtype=jnp.int32), rows)
    ang_swa = _axial_angles(row, col, HEAD_DIM)
    ang_mla = _axial_angles(row, col, MLA_ROPE)
    lbp = jax.nn.softmax(hgrn_lb_logits.astype(F32), axis=0)
    lower_bounds = jnp.cumsum(lbp, axis=0) - lbp[0]

    for l in range(DEPTH):
        need_ctx = l < DEPTH - 1
        ml = (jax.nn.silu(c) @ w_ada[l] + b_ada[l])[:, None, :]
        mc = jax.nn.silu(c_ctx) @ w_ada[l] + b_ada[l]
        sh1, sc1, g1, sh2, sc2, g2 = jnp.split(ml, 6, axis=-1)
        csh1, csc1, cg1, csh2, csc2, cg2 = jnp.split(mc, 6, axis=-1)

        o_lat, o_ctx = _token_mixers(_modulate(x, sh1, sc1), _modulate(ctx, csh1, csc1), w_in[l], w_out[l],
                                     swa_sink[l], ret_decay_exp[l], ret_gn[l], mla_q_norm[l], mla_kv_norm[l],
                                     mla_w_uq[l], mla_w_ukv[l], lower_bounds[l], hgrn_gn[l], ang_swa, ang_mla, need_ctx)
        x = _layer_norm(DEEPNORM_ALPHA * x + g1 * o_lat, ln1_g[l], ln1_b[l])
        h2 = _modulate(x, sh2, sc2).reshape(B * T, D)
        if need_ctx:
            ctx = _layer_norm(DEEPNORM_ALPHA * ctx + cg1 * o_ctx, ln1_g[l], ln1_b[l])
            h2c = _modulate(ctx, csh2, csc2).reshape(B * L, D)
            y = _hier_moe(jnp.concatenate([h2, h2c], 0), router_group[l], router_expert[l],
                          moe_w1[l], moe_w3[l], moe_w2[l])
            y_lat, y_ctx = y[:B * T].reshape(B, T, D), y[B * T:].reshape(B, L, D)
            ctx = _layer_norm(DEEPNORM_ALPHA * ctx + cg2 * y_ctx, ln2_g[l], ln2_b[l])
        else:
            y_lat = _hier_moe(h2, router_group[l], router_expert[l], moe_w1[l], moe_w3[l], moe_w2[l]).reshape(B, T, D)
        x = _layer_norm(DEEPNORM_ALPHA * x + g2 * y_lat, ln2_g[l], ln2_b[l])
    return x
```

```python
import math
import numpy as np
import concourse.bass as bass
import concourse.mybir as mybir
from concourse.bass_utils import run_bass_kernel_spmd
from contextlib import ExitStack

F32 = mybir.dt.float32
BF16 = mybir.dt.bfloat16
AF = mybir.ActivationFunctionType
ALU = mybir.AluOpType
AX = mybir.AxisListType

GEN = 12000
D = 2048
T = 4096
L = 256
NTOK = T + L
NTILE = NTOK // 128
EPS = 1e-6
ALPHA = (2 * 2) ** 0.25
NWIN = 2112


class Prog:
    ENG = ('pe', 'act', 'dve', 'pool', 'sp')

    def __init__(self, nc, stack, n_dma_sems=24):
        self.nc = nc
        self.stack = stack
        self.streams = {e: [] for e in self.ENG}
        self.count = {e: 0 for e in self.ENG}
        self.sems = {}
        self.n_dma = n_dma_sems
        self.dma_val = [0] * n_dma_sems
        self.dma_rr = 0
        self.last_write = {}
        self.readers = {}
        self.waited = {e: {} for e in self.ENG}

    def sem(self, key):
        if key not in self.sems:
            name = "s_" + "_".join(str(k) for k in key)
            self.sems[key] = self.stack.enter_context(self.nc.semaphore(name))
        return self.sems[key]

    def op(self, eng, fn, reads=(), writes=(), dma=False):
        deps = {}

        def add(tok):
            if tok is None:
                return
            k, v, te = tok
            if eng == 'pe' and te == 'pe' and k[0] == 'c':
                return
            if deps.get(k, 0) < v:
                deps[k] = v
        for r in reads:
            add(self.last_write.get(r))
        for w in writes:
            add(self.last_write.get(w))
            for k, (v, te) in self.readers.get(w, {}).items():
                add((k, v, te))
        if dma:
            j = self.dma_rr
            self.dma_rr = (self.dma_rr + 1) % self.n_dma
            if self.dma_val[j] > 0:
                add((('d', j), self.dma_val[j], 'dma'))
            self.dma_val[j] += 16
            tok = (('d', j), self.dma_val[j], 'dma')
        else:
            idx = self.count[eng]
            self.count[eng] += 1
            tok = (('c', eng, idx // GEN), idx % GEN + 1, eng)
        waits = []
        wd = self.waited[eng]
        for k, v in deps.items():
            if wd.get(k, 0) >= v:
                continue
            wd[k] = v
            waits.append((k, v))
            self.sem(k)
        self.sem(tok[0])
        self.streams[eng].append((waits, fn, tok, dma))
        for r in reads:
            d = self.readers.setdefault(r, {})
            if d.get(tok[0], (0, None))[0] < tok[1]:
                d[tok[0]] = (tok[1], tok[2])
        for w in writes:
            self.last_write[w] = tok
            self.readers[w] = {}
        return tok

    def _all_waits(self, e):
        waits = []
        wd = self.waited[e]
        for j in range(self.n_dma):
            if self.dma_val[j] > 0 and wd.get(('d', j), 0) < self.dma_val[j]:
                wd[('d', j)] = self.dma_val[j]
                waits.append((('d', j), self.dma_val[j]))
        for e2 in self.ENG:
            n = self.count[e2]
            if n > 0:
                k = ('c', e2, (n - 1) // GEN)
                v = (n - 1) % GEN + 1
                if wd.get(k, 0) < v:
                    wd[k] = v
                    waits.append((k, v))
        return waits

    def finish(self):
        self.streams['sp'].append((self._all_waits('sp'), None, None, False))

    def barrier(self):
        for e in self.ENG:
            w = self._all_waits(e)
            if w:
                self.streams[e].append((w, None, None, False))

    def emit(self):
        nc = self.nc
        P = self
        with nc.Block() as block:
            def run(engname):
                def body(e):
                    for waits, fn, tok, dma in P.streams[engname]:
                        for k, v in waits:
                            e.wait_ge(P.sems[k], v)
                        if fn is None:
                            continue
                        ins = fn(e)
                        ins.then_inc(P.sems[tok[0]], 16 if dma else 1)
                return body
            block.tensor(run('pe'))
            block.scalar(run('act'))
            block.vector(run('dve'))
            block.gpsimd(run('pool'))
            block.sync(run('sp'))
        self.streams = {e: [] for e in self.ENG}


class KB:
    def __init__(self, nc, es):
        self.nc = nc
        self.es = es
        self.P = Prog(nc, es)
        self.psf = [es.enter_context(nc.psum_tensor("psf%d" % i, [128, 512], F32)) for i in range(6)]
        self.psb = [es.enter_context(nc.psum_tensor("psb%d" % i, [128, 1024], BF16)) for i in range(2)]
        self.slot_rr = 0
        self.bank_rr = 0
        self.ph = None

    def phase(self):
        self.ph = ExitStack()
        return self.ph

    def sb(self, name, shape, dt):
        self.uid = getattr(self, 'uid', 0) + 1
        return self.ph.enter_context(self.nc.sbuf_tensor("%s_u%d" % (name, self.uid), shape, dt))

    def end_phase(self, last=False):
        if last:
            self.P.finish()
        else:
            self.P.barrier()
        self.P.emit()

    def pslot(self, banks=(0, 1, 2, 3, 4, 5)):
        i = self.slot_rr
        self.slot_rr += 1
        b = banks[i % len(banks)]
        return self.psf[b][:, 0:128], ["ps%d_%d" % (b, s) for s in range(4)]

    def pbank(self, banks=(0, 1, 2, 3, 4, 5)):
        i = self.bank_rr % len(banks)
        self.bank_rr += 1
        b = banks[i]
        return self.psf[b], ["ps%d_%d" % (b, s) for s in range(4)]

    def dma(self, out, in_, reads=(), writes=(), q='sp'):
        self.P.op(q, lambda e: e.dma_start(out=out, in_=in_), reads, writes, dma=True)

    def mm(self, out, lhsT, rhs, start, stop, reads=(), writes=()):
        self.P.op('pe', lambda e: e.matmul(out, lhsT=lhsT, rhs=rhs, start=start, stop=stop), reads, writes)

    def tr(self, out, in_, ident, reads=(), writes=()):
        self.P.op('pe', lambda e: e.transpose(out=out, in_=in_, identity=ident), reads, writes)

    def act(self, out, in_, func, reads=(), writes=(), bias=None, scale=None):
        kw = {}
        if bias is not None:
            kw['bias'] = bias
        if scale is not None:
            kw['scale'] = scale
        self.P.op('act', lambda e: e.activation(out=out, in_=in_, func=func, **kw), reads, writes)

    def tt(self, eng, out, in0, in1, op, reads=(), writes=()):
        self.P.op(eng, lambda e: e.tensor_tensor(out=out, in0=in0, in1=in1, op=op), reads, writes)

    def ts(self, eng, out, in0, s1, op0, reads=(), writes=(), s2=None, op1=None):
        if op1 is None:
            self.P.op(eng, lambda e: e.tensor_scalar(out=out, in0=in0, scalar1=s1, scalar2=None, op0=op0), reads, writes)
        else:
            self.P.op(eng, lambda e: e.tensor_scalar(out=out, in0=in0, scalar1=s1, scalar2=s2, op0=op0, op1=op1), reads, writes)

    def stt(self, eng, out, in0, scalar, in1, op0, op1, reads=(), writes=()):
        self.P.op(eng, lambda e: e.scalar_tensor_tensor(out=out, in0=in0, scalar=scalar, in1=in1, op0=op0, op1=op1), reads, writes)

    def cp(self, eng, out, in_, reads=(), writes=()):
        if eng == 'act':
            self.P.op('act', lambda e: e.copy(out=out, in_=in_), reads, writes)
        else:
            self.P.op(eng, lambda e: e.tensor_copy(out=out, in_=in_), reads, writes)

    def recip(self, out, in_, reads=(), writes=()):
        self.P.op('dve', lambda e: e.reciprocal(out=out, in_=in_), reads, writes)

    def rmax(self, out, in_, reads=(), writes=()):
        self.P.op('dve', lambda e: e.reduce_max(out=out, in_=in_, axis=AX.X), reads, writes)

    def memset(self, eng, ap, val, writes=()):
        self.P.op(eng, lambda e: e.memset(ap, val), (), writes)

    def asel(self, out, in_, pattern, op, fill, base, cm, reads=(), writes=()):
        self.P.op('pool', lambda e: e.affine_select(out=out, in_=in_, pattern=pattern, compare_op=op, fill=fill,
                                                    base=base, channel_multiplier=cm), reads, writes)

    def iota(self, out, pattern, base, cm, writes=()):
        self.P.op('pool', lambda e: e.iota(out, pattern=pattern, base=base, channel_multiplier=cm,
                                           allow_small_or_imprecise_dtypes=True), (), writes)

    def rsqrt(self, out, in_, mul, reads=(), writes=()):
        self.ts('dve', out, in_, mul, ALU.mult, reads, writes, s2=EPS, op1=ALU.add)
        self.act(out, out, AF.Sqrt, writes, writes)
        self.recip(out, out, writes, writes)


C_SQ, C_SK, C_SV, C_RQ, C_RK, C_RV, C_RG, C_CQ, C_CKV, C_KR, C_HQ, C_HFF, C_HFB, C_HI, C_HG = (
    0, 128, 256, 384, 512, 640, 768, 896, 1280, 1408, 1472, 1600, 1728, 1856, 1984)
FM_CHUNKS = [("sq", C_SQ, 128), ("sk", C_SK, 128), ("rq", C_RQ, 128), ("rk", C_RK, 128), ("rg", C_RG, 128),
             ("cq0", C_CQ, 128), ("cq1", C_CQ + 128, 128), ("cq2", C_CQ + 256, 128), ("ckv", C_CKV, 128),
             ("kr", C_KR, 64), ("hq", C_HQ, 128), ("hff", C_HFF, 128), ("hfb", C_HFB, 128), ("hg", C_HG, 128)]
FM_IDX = {n: i for i, (n, _, _) in enumerate(FM_CHUNKS)}
TM_SETS = [("sv", C_SV), ("rv", C_RV), ("rk", C_RK), ("hi", C_HI)]
TM_IDX = {n: i for i, (n, _) in enumerate(TM_SETS)}
GROUPS = [(g * 512, min(512, NTOK - g * 512)) for g in range(9)]


def build_A(layer=0, upto=99, dbg=False):
    nc = bass.Bass("TRN2", target_bir_lowering=False)
    dt_in = lambda n, s: nc.dram_tensor(n, s, F32, kind="ExternalInput").ap()
    xin = dt_in("xin", [NTOK, D])
    cT = dt_in("cT", [128, 32])
    wada = dt_in("wada", [D, 4096])
    badaT = dt_in("badaT", [128, 32])
    win = dt_in("win", [D, NWIN])
    ropeAc = dt_in("ropeAc", [128, NTOK])
    ropeAs = dt_in("ropeAs", [128, NTOK])
    ropeMc = dt_in("ropeMc", [64, NTOK])
    ropeMs = dt_in("ropeMs", [64, NTOK])
    sm = dt_in("sm", [128, 16])
    wuq = dt_in("wuq", [384, 256])
    wukv = dt_in("wukv", [128, 256])
    mixT = nc.dram_tensor("mixT", [512, NTOK], F32, kind="ExternalOutput").ap()
    dk = dict(kind="ExternalOutput") if dbg else {}
    projT = nc.dram_tensor("projT", [len(FM_CHUNKS), 128, NTOK], F32, **dk).ap()
    projV = nc.dram_tensor("projV", [len(TM_SETS), NTOK, 128], F32, **dk).ap()
    if dbg:
        modD = nc.dram_tensor("modD", [128, 64], F32, kind="ExternalOutput").ap()

    with ExitStack() as es:
        kb = KB(nc, es)
        P = kb.P
        gsb = lambda n, s, d: es.enter_context(nc.sbuf_tensor(n, s, d))
        ones_f = gsb("ones_f", [128, 128], F32)
        ones_b = gsb("ones_b", [128, 128], BF16)
        ident_f = gsb("ident_f", [128, 128], F32)
        ident_b = gsb("ident_b", [128, 128], BF16)
        smt = gsb("smt", [128, 16], F32)
        modT = gsb("modT", [128, 32, 2], F32)
        kb.memset('pool', ones_f[:], 1.0, ['ones_f'])
        kb.cp('dve', ones_b[:], ones_f[:], ['ones_f'], ['ones_b'])
        kb.memset('pool', ident_f[:], 1.0, ['ident_f'])
        kb.asel(ident_f[:], ident_f[:], [[-1, 128]], ALU.is_equal, 0.0, 0, 1, ['ident_f'], ['ident_f'])
        kb.cp('dve', ident_b[:], ident_f[:], ['ident_f'], ['ident_b'])
        kb.dma(smt[:], sm[:, :], (), ['smt'])

        with kb.phase():
            cTs = kb.sb("cTs", [128, 32], F32)
            sT = kb.sb("sT", [128, 32], F32)
            bT = kb.sb("bT", [128, 32], F32)
            wk = [kb.sb("wk%d" % i, [128, 4096], F32) for i in range(2)]
            kb.dma(cTs[:], cT[:, :], (), ['cTs'])
            kb.dma(bT[:], badaT[:, :], (), ['bT'])
            kb.act(sT[:], cTs[:], AF.Silu, ['cTs'], ['sT'])
            pb = kb.psf[0]
            pbn = ["ps0_%d" % s for s in range(4)]
            for k in range(16):
                w = wk[k % 2]
                wn = 'wk%d' % (k % 2)
                kb.dma(w[:], wada[k * 128:(k + 1) * 128, :], (), [wn], q='sp' if k % 2 == 0 else 'act')
                for j in range(32):
                    kb.mm(pb[:, 2 * j:2 * j + 2], w[:, j * 128:(j + 1) * 128], sT[:, 2 * k:2 * k + 2],
                          k == 0 and j == 0, k == 15, [wn, 'sT'], pbn)
            kb.tt('dve', modT[:], pb[:, 0:64].rearrange("p (j r) -> p j r", r=2),
                  bT[:].unsqueeze(2).to_broadcast([128, 32, 2]), ALU.add, pbn + ['bT'], ['modT'])
            kb.ts('dve', modT[:, 16:32, :], modT[:, 16:32, :], 1.0, ALU.add, ['modT'], ['modT'])
            if dbg:
                kb.dma(modD[:, :], modT[:].rearrange("p j r -> p (j r)"), ['modT'], ['modD'])
            kb.end_phase(upto == 0)
            if upto == 0:
                return nc

        with kb.phase():
            winb = kb.sb("winb", [128, 16, NWIN], BF16)
            for k in range(16):
                kb.dma(winb[:, k, :], win[k * 128:(k + 1) * 128, :], (), ['winb%d' % k], q='pool')
            winb_r = ['winb%d' % k for k in range(16)]
            xb = [kb.sb("xb%d" % i, [128, D], F32) for i in range(2)]
            xnb = [kb.sb("xnb%d" % i, [128, D], BF16) for i in range(2)]
            hT = [kb.sb("hT%d" % i, [128, 16, 512], BF16) for i in range(2)]
            stats = kb.sb("stats", [128, 4, 6], F32)
            mv = kb.sb("mv", [128, 4], F32)
            stg = [kb.sb("stg%d" % i, [128, 512], F32) for i in range(3)]
            stgv = [kb.sb("stgv%d" % i, [128, 4, 128], F32) for i in range(2)]
            stg_rr = 0
            for t in range(NTILE):
                g, s = t // 4, t % 4
                r = 1 if t < 2 else 0
                x_ = xb[t % 2]
                xn = 'xb%d' % (t % 2)
                kb.dma(x_[:], xin[t * 128:(t + 1) * 128, :], (), [xn])
                for c in range(4):
                    P.op('dve', lambda e, c=c, x_=x_: e.bn_stats(out=stats[:, c, :], in_=x_[:, c * 512:(c + 1) * 512]),
                         [xn], ['stats'])
                P.op('dve', lambda e: e.bn_aggr(out=mv[:, 0:2], in_=stats[:]), ['stats'], ['mv'])
                kb.rsqrt(mv[:, 1:2], mv[:, 1:2], 1.0, ['mv'], ['mv'])
                kb.ts('dve', mv[:, 2:3], mv[:, 0:1], mv[:, 1:2], ALU.mult, ['mv'], ['mv'], s2=-1.0, op1=ALU.mult)
                xq = xnb[t % 2]
                xqn = 'xnb%d' % (t % 2)
                kb.act(xq[:], x_[:], AF.Identity, [xn, 'mv'], [xqn], bias=mv[:, 2:3], scale=mv[:, 1:2])
                h_ = hT[g % 2]
                hn = 'hT%d' % (g % 2)
                for half in range(2):
                    pbb = kb.psb[half]
                    pbbn = ['psb%d' % half]
                    for kk in range(8):
                        k = half * 8 + kk
                        kb.tr(pbb[:, kk * 128:(kk + 1) * 128], xq[:, k * 128:(k + 1) * 128], ident_b[:],
                              [xqn, 'ident_b'], pbbn)
                    for kk in range(8):
                        k = half * 8 + kk
                        o = h_[:, k, s * 128:(s + 1) * 128]
                        i_ = pbb[:, kk * 128:(kk + 1) * 128]
                        if kk % 2 == 0:
                            kb.ts('dve', o, i_, modT[:, 16 + k, r:r + 1], ALU.mult, pbbn + ['modT'], [hn + '_%d' % s],
                                  s2=modT[:, k, r:r + 1], op1=ALU.add)
                        else:
                            kb.act(o, i_, AF.Identity, pbbn + ['modT'], [hn + '_%d' % s],
                                   bias=modT[:, k, r:r + 1], scale=modT[:, 16 + k, r:r + 1])
                last = (s == 3) or (t == NTILE - 1)
                if not last:
                    continue
                g0, n = GROUPS[g]
                hr = [hn + '_%d' % s2 for s2 in range(n // 128)]
                for ci, (cn, off, width) in enumerate(FM_CHUNKS):
                    pb, pbn = kb.pbank((0, 1, 2, 3))
                    for k in range(16):
                        kb.mm(pb[:width, :n], winb[:, k, off:off + width], h_[:, k, :n], k == 0, k == 15,
                              hr + [winb_r[k]], pbn)
                    st = stg[stg_rr % 3]
                    stn = 'stg%d' % (stg_rr % 3)
                    stg_rr += 1
                    kb.cp('act' if ci % 2 == 0 else 'dve', st[:width, :n], pb[:width, :n], pbn, [stn])
                    kb.dma(projT[ci, 0:width, g0:g0 + n], st[:width, :n], [stn], ['projT_%s_%d' % (cn, g)])
                for vi, (vn, off) in enumerate(TM_SETS):
                    pb, pbn = kb.pbank((4, 5))
                    nt = n // 128
                    for s2 in range(nt):
                        for k in range(16):
                            kb.mm(pb[:, s2 * 128:(s2 + 1) * 128], h_[:, k, s2 * 128:(s2 + 1) * 128],
                                  winb[:, k, off:off + 128], k == 0, k == 15, hr + [winb_r[k]], pbn)
                    sv = stgv[vi % 2]
                    svn = 'stgv%d' % (vi % 2)
                    kb.cp('dve' if vi % 2 == 0 else 'act', sv[:, 0:nt, :],
                          pb[:, 0:nt * 128].rearrange("p (s c) -> p s c", c=128), pbn, [svn])
                    kb.dma(projV[vi, g0:g0 + n, :].rearrange("(s p) c -> p s c", p=128), sv[:, 0:nt, :], [svn],
                           ['projV_%s_%d' % (vn, g)])
            kb.end_phase(upto == 1)
            if upto == 1:
                return nc

        fm_r = lambda cn: ['projT_%s_%d' % (cn, g) for g in range(9)]
        tm_r = lambda vn: ['projV_%s_%d' % (vn, g) for g in range(9)]

        def load_fm(dst, cn, rows=128, rot=None, q='sp', wname=None):
            ci = FM_IDX[cn]
            if rot is None:
                kb.dma(dst[0:rows, :], projT[ci, 0:rows, :], fm_r(cn), [wname], q=q)
            else:
                kb.dma(dst[0:rot, :], projT[ci, rot:2 * rot, :], fm_r(cn), [wname], q=q)
                kb.dma(dst[rot:2 * rot, :], projT[ci, 0:rot, :], fm_r(cn), [wname], q=q)

        def load_tm(dst, vn, wname, q='sp'):
            kb.dma(dst[:], projV[TM_IDX[vn], :, :].rearrange("(t p) c -> p t c", p=128), tm_r(vn), [wname], q=q)

        def norm_bound(src_list, outcol, nm):
            sq = kb.sb("nb_sq_" + nm, [128, 512], BF16)
            mx = kb.sb("nb_mx_" + nm, [128, 16], F32)
            for g, (g0, n) in enumerate(GROUPS):
                pb, pbn = kb.pbank((4, 5))
                for i, (src, rows, rn) in enumerate(src_list):
                    kb.tt('dve', sq[0:rows, :n], src[0:rows, g0:g0 + n], src[0:rows, g0:g0 + n], ALU.mult,
                          [rn], ['nb_sq' + nm])
                    kb.mm(pb[:, :n], ones_b[0:rows, :], sq[0:rows, :n], i == 0, i == len(src_list) - 1,
                          ['nb_sq' + nm, 'ones_b'], pbn)
                kb.rmax(mx[:, g:g + 1], pb[:, :n], pbn, ['nb_mx' + nm])
            kb.rmax(outcol, mx[:, 0:9], ['nb_mx' + nm], [nm])

        def rope_fm(dst_b, x, xr, ct, st, rows, rn_x, rn_xr, rn_dst, tmp):
            kb.tt('dve', x[0:rows, :], x[0:rows, :], ct[0:rows, :], ALU.mult, [rn_x, 'ropetab'], [rn_x])
            kb.tt('pool', xr[0:rows, :], xr[0:rows, :], st[0:rows, :], ALU.mult, [rn_xr, 'ropetab'], [rn_xr])
            kb.tt('dve', dst_b[0:rows, :], x[0:rows, :], xr[0:rows, :], ALU.add, [rn_x, rn_xr], [rn_dst])

        with kb.phase():
            sc = 128 ** -0.5
            ct = kb.sb("a_ct", [128, NTOK], F32)
            st_ = kb.sb("a_st", [128, NTOK], F32)
            x0 = kb.sb("a_x0", [128, NTOK], F32)
            x1 = kb.sb("a_x1", [128, NTOK], F32)
            qTb = kb.sb("a_qTb", [128, NTOK], BF16)
            kTb = kb.sb("a_kTb", [128, NTOK], BF16)
            vf = kb.sb("a_vf", [128, NTILE, 128], F32)
            vb = kb.sb("a_vb", [128, NTILE, 128], BF16)
            oA = kb.sb("a_o", [128, NTOK], F32)
            mP = kb.sb("a_mP", [128, 128], BF16)
            mN = kb.sb("a_mN", [128, 128], BF16)
            mtmp = kb.sb("a_mtmp", [128, 128], F32)
            bnd = kb.sb("a_bnd", [128, 8], F32)
            pT = [kb.sb("a_pT%d" % i, [128, 128], BF16) for i in range(4)]
            dn = [kb.sb("a_dn%d" % i, [128, 128], F32) for i in range(2)]
            kb.dma(ct[:], ropeAc[:, :], (), ['ropetab'])
            kb.dma(st_[:], ropeAs[:, :], (), ['ropetab'])
            load_fm(x0, "sq", wname='a_x0')
            load_fm(x1, "sq", rot=64, wname='a_x1', q='act')
            rope_fm(qTb, x0, x1, ct, st_, 128, 'a_x0', 'a_x1', 'a_qTb', None)
            load_fm(x0, "sk", wname='a_x0')
            load_fm(x1, "sk", rot=64, wname='a_x1', q='act')
            rope_fm(kTb, x0, x1, ct, st_, 128, 'a_x0', 'a_x1', 'a_kTb', None)
            load_tm(vf, "sv", 'a_vf')
            kb.cp('act', vb[:], vf[:], ['a_vf'], ['a_vb'])
            kb.memset('pool', mtmp[:], 1.0, ['a_mtmp'])
            kb.asel(mtmp[:], mtmp[:], [[-1, 128]], ALU.is_ge, 0.0, 0, 1, ['a_mtmp'], ['a_mtmp'])
            kb.cp('dve', mP[:], mtmp[:], ['a_mtmp'], ['a_mP'])
            kb.memset('pool', mtmp[:], 1.0, ['a_mtmp'])
            kb.asel(mtmp[:], mtmp[:], [[1, 128]], ALU.is_ge, 0.0, 0, -1, ['a_mtmp'], ['a_mtmp'])
            kb.cp('dve', mN[:], mtmp[:], ['a_mtmp'], ['a_mN'])
            norm_bound([(qTb, 128, 'a_qTb')], bnd[:, 0:1], 'a_bq')
            norm_bound([(kTb, 128, 'a_kTb')], bnd[:, 1:2], 'a_bk')
            kb.tt('dve', bnd[:, 2:3], bnd[:, 0:1], bnd[:, 1:2], ALU.mult, ['a_bq', 'a_bk'], ['a_bnd'])
            kb.act(bnd[:, 2:3], bnd[:, 2:3], AF.Sqrt, ['a_bnd'], ['a_bnd'])
            kb.ts('dve', bnd[:, 3:4], bnd[:, 2:3], -1.02 * sc, ALU.mult, ['a_bnd'], ['a_bnd'])
            kb.act(bnd[:, 4:5], smt[:, 0:1], AF.Exp, ['smt', 'a_bnd'], ['a_es'], bias=bnd[:, 3:4], scale=1.0)
            rr = 0
            for tq in range(NTILE):
                keys = [0, 1]
                if tq >= 2:
                    keys += [j for j in (tq - 1, tq, tq + 1) if 2 <= j <= NTILE - 1]
                pO, pOn = kb.psf[tq % 2][:, 0:128], ["ps%d_%d" % (tq % 2, s) for s in range(4)]
                pD, pDn = kb.psf[2 + tq % 2][:, 0:128], ["ps%d_%d" % (2 + tq % 2, s) for s in range(4)]
                for i, j in enumerate(keys):
                    pS, pSn = kb.pslot((4, 5))
                    kb.mm(pS, kTb[:, j * 128:(j + 1) * 128], qTb[:, tq * 128:(tq + 1) * 128], True, True,
                          ['a_kTb', 'a_qTb'], pSn)
                    p_ = pT[rr % 4]
                    pn = 'a_pT%d' % (rr % 4)
                    rr += 1
                    kb.act(p_[:], pS, AF.Exp, pSn + ['a_bnd'], [pn], bias=bnd[:, 3:4], scale=sc)
                    if tq >= 2 and j >= 2 and j == tq - 1:
                        kb.tt('dve', p_[:], p_[:], mP[:], ALU.mult, [pn, 'a_mP'], [pn])
                    if tq >= 2 and j == tq + 1:
                        kb.tt('dve', p_[:], p_[:], mN[:], ALU.mult, [pn, 'a_mN'], [pn])
                    kb.mm(pO, vb[:, j, :], p_[:], i == 0, i == len(keys) - 1, ['a_vb', pn], pOn)
                    kb.mm(pD, ones_b[:], p_[:], i == 0, i == len(keys) - 1, ['ones_b', pn], pDn)
                d_ = dn[tq % 2]
                dnn = 'a_dn%d' % (tq % 2)
                kb.ts('dve', d_[:], pD, bnd[:, 4:5], ALU.add, pDn + ['a_es'], [dnn])
                kb.recip(d_[:], d_[:], [dnn], [dnn])
                kb.tt('dve', oA[:, tq * 128:(tq + 1) * 128], pO, d_[:], ALU.mult, pOn + [dnn], ['a_o'])
            kb.dma(mixT[0:128, :], oA[:], ['a_o'], ['mixT_a'])
            kb.end_phase(upto == 2)
            if upto == 2:
                return nc

        with kb.phase():
            sc = 192 ** -0.5
            wuqb = kb.sb("c_wuqb", [128, 3, 256], BF16)
            wukvb = kb.sb("c_wukvb", [128, 256], BF16)
            for k in range(3):
                kb.dma(wuqb[:, k, :], wuq[k * 128:(k + 1) * 128, :], (), ['c_wuqb'], q='pool')
            kb.dma(wukvb[:], wukv[:, :], (), ['c_wukvb'], q='pool')
            cmt = kb.sb("c_cmt", [64, NTOK], F32)
            smt_ = kb.sb("c_smt", [64, NTOK], F32)
            kb.dma(cmt[:], ropeMc[:, :], (), ['ropetab'])
            kb.dma(smt_[:], ropeMs[:, :], (), ['ropetab'])
            qnT = kb.sb("c_qnT", [128, NTOK], BF16)
            qrT = kb.sb("c_qrT", [64, NTOK], BF16)
            knT = kb.sb("c_knT", [128, NTOK], BF16)
            krT = kb.sb("c_krT", [64, NTOK], BF16)
            vM = kb.sb("c_vM", [128, NTILE, 128], BF16)
            kx0 = kb.sb("c_kx0", [64, NTOK], F32)
            kx1 = kb.sb("c_kx1", [64, NTOK], F32)
            load_fm(kx0, "kr", rows=64, wname='c_kx0')
            load_fm(kx1, "kr", rot=32, wname='c_kx1', q='act')
            rope_fm(krT, kx0, kx1, cmt, smt_, 64, 'c_kx0', 'c_kx1', 'c_krT', None)
            cq = [kb.sb("c_cq%d" % i, [128, 3, 512], F32) for i in range(2)]
            cqs = kb.sb("c_cqs", [128, 3, 512], BF16)
            cqn = kb.sb("c_cqn", [128, 3, 512], BF16)
            rs = kb.sb("c_rs", [128, 512], F32)
            ckv = [kb.sb("c_ckv%d" % i, [128, 512], F32) for i in range(2)]
            ckvs = kb.sb("c_ckvs", [128, 512], BF16)
            ckvn = kb.sb("c_ckvn", [128, 512], BF16)
            rs2 = kb.sb("c_rs2", [128, 512], F32)
            qr0 = kb.sb("c_qr0", [64, 512], F32)
            qr1 = kb.sb("c_qr1", [64, 512], F32)
            for g, (g0, n) in enumerate(GROUPS):
                cq_ = cq[g % 2]
                cqn_ = 'c_cq%d' % (g % 2)
                for k in range(3):
                    kb.dma(cq_[:, k, :n], projT[FM_IDX["cq%d" % k], :, g0:g0 + n], fm_r("cq%d" % k), [cqn_],
                           q='sp' if k != 1 else 'act')
                kb.tt('dve', cqs[:, :, :n], cq_[:, :, :n], cq_[:, :, :n], ALU.mult, [cqn_], ['c_cqs'])
                pb, pbn = kb.pbank((0, 1, 2, 3))
                for k in range(3):
                    kb.mm(pb[:, :n], ones_b[:], cqs[:, k, :n], k == 0, k == 2, ['ones_b', 'c_cqs'], pbn)
                kb.rsqrt(rs[:, :n], pb[:, :n], 1.0 / 384, pbn, ['c_rs'])
                for k in range(3):
                    kb.stt('dve', cqn[:, k, :n], cq_[:, k, :n], smt[:, 5 + k:6 + k], rs[:, :n],
                           ALU.mult, ALU.mult, [cqn_, 'smt', 'c_rs'], ['c_cqn'])
                pb, pbn = kb.pbank((0, 1, 2, 3))
                for k in range(3):
                    kb.mm(pb[:, :n], wuqb[:, k, 0:128], cqn[:, k, :n], k == 0, k == 2, ['c_wuqb', 'c_cqn'], pbn)
                kb.cp('act', qnT[:, g0:g0 + n], pb[:, :n], pbn, ['c_qnT'])
                pb, pbn = kb.pbank((0, 1, 2, 3))
                for k in range(3):
                    kb.mm(pb[0:64, :n], wuqb[:, k, 128:192], cqn[:, k, :n], k == 0, k == 2, ['c_wuqb', 'c_cqn'], pbn)
                kb.tt('dve', qr0[:, :n], pb[0:64, :n], cmt[:, g0:g0 + n], ALU.mult, pbn + ['ropetab'], ['c_qr0'])
                pb, pbn = kb.pbank((0, 1, 2, 3))
                for k in range(3):
                    kb.mm(pb[0:64, :n], wuqb[:, k, 192:256], cqn[:, k, :n], k == 0, k == 2, ['c_wuqb', 'c_cqn'], pbn)
                kb.tt('dve', qr1[:, :n], pb[0:64, :n], smt_[:, g0:g0 + n], ALU.mult, pbn + ['ropetab'], ['c_qr1'])
                kb.tt('pool', qrT[:, g0:g0 + n], qr0[:, :n], qr1[:, :n], ALU.add, ['c_qr0', 'c_qr1'], ['c_qrT'])
                ck_ = ckv[g % 2]
                ckn_ = 'c_ckv%d' % (g % 2)
                kb.dma(ck_[:, :n], projT[FM_IDX["ckv"], :, g0:g0 + n], fm_r("ckv"), [ckn_], q='act')
                kb.tt('pool', ckvs[:, :n], ck_[:, :n], ck_[:, :n], ALU.mult, [ckn_], ['c_ckvs'])
                pb, pbn = kb.pbank((0, 1, 2, 3))
                kb.mm(pb[:, :n], ones_b[:], ckvs[:, :n], True, True, ['ones_b', 'c_ckvs'], pbn)
                kb.rsqrt(rs2[:, :n], pb[:, :n], 1.0 / 128, pbn, ['c_rs2'])
                kb.stt('dve', ckvn[:, :n], ck_[:, :n], smt[:, 4:5], rs2[:, :n], ALU.mult, ALU.mult,
                       [ckn_, 'smt', 'c_rs2'], ['c_ckvn'])
                pb, pbn = kb.pbank((0, 1, 2, 3))
                kb.mm(pb[:, :n], wukvb[:, 0:128], ckvn[:, :n], True, True, ['c_wukvb', 'c_ckvn'], pbn)
                kb.cp('act', knT[:, g0:g0 + n], pb[:, :n], pbn, ['c_knT'])
                pb, pbn = kb.pbank((4, 5))
                nt = n // 128
                for s2 in range(nt):
                    kb.mm(pb[:, s2 * 128:(s2 + 1) * 128], ckvn[:, s2 * 128:(s2 + 1) * 128], wukvb[:, 128:256], True, True,
                          ['c_wukvb', 'c_ckvn'], pbn)
                kb.cp('act', vM[:, g * 4:g * 4 + nt, :], pb[:, 0:nt * 128].rearrange("p (s c) -> p s c", c=128), pbn,
                      ['c_vM'])
            bnd = kb.sb("c_bnd", [128, 8], F32)
            norm_bound([(qnT, 128, 'c_qnT'), (qrT, 64, 'c_qrT')], bnd[:, 0:1], 'c_bq')
            norm_bound([(knT, 128, 'c_knT'), (krT, 64, 'c_krT')], bnd[:, 1:2], 'c_bk')
            kb.tt('dve', bnd[:, 2:3], bnd[:, 0:1], bnd[:, 1:2], ALU.mult, ['c_bq', 'c_bk'], ['c_bnd'])
            kb.act(bnd[:, 2:3], bnd[:, 2:3], AF.Sqrt, ['c_bnd'], ['c_bnd'])
            kb.ts('dve', bnd[:, 3:4], bnd[:, 2:3], -1.02 * sc, ALU.mult, ['c_bnd'], ['c_bnd'])
            pTc = [kb.sb("c_pT%d" % i, [128, 512], BF16) for i in range(3)]
            rd = [kb.sb("c_rd%d" % i, [128, 512], F32) for i in range(2)]
            oc = [kb.sb("c_oc%d" % i, [128, 512], F32) for i in range(2)]
            qgroups = [(0, 256, [0, 1])] + [(256 + i * 512, 512, list(range(NTILE))) for i in range(8)]
            rr = 0
            for gi, (q0, n, keys) in enumerate(qgroups):
                pO, pOn = (kb.psf[gi % 2], ["ps%d_%d" % (gi % 2, s) for s in range(4)])
                pD, pDn = (kb.psf[2 + gi % 2], ["ps%d_%d" % (2 + gi % 2, s) for s in range(4)])
                for i, j in enumerate(keys):
                    pS, pSn = kb.pbank((4, 5))
                    kb.mm(pS[:, :n], knT[:, j * 128:(j + 1) * 128], qnT[:, q0:q0 + n], True, False,
                          ['c_knT', 'c_qnT'], pSn)
                    kb.mm(pS[:, :n], krT[:, j * 128:(j + 1) * 128], qrT[:, q0:q0 + n], False, True,
                          ['c_krT', 'c_qrT'], pSn)
                    p_ = pTc[rr % 3]
                    pn = 'c_pT%d' % (rr % 3)
                    rr += 1
                    kb.act(p_[:, :n], pS[:, :n], AF.Exp, pSn + ['c_bnd'], [pn], bias=bnd[:, 3:4], scale=sc)
                    kb.mm(pO[:, :n], vM[:, j, :], p_[:, :n], i == 0, i == len(keys) - 1, ['c_vM', pn], pOn)
                    kb.mm(pD[:, :n], ones_b[:], p_[:, :n], i == 0, i == len(keys) - 1, ['ones_b', pn], pDn)
                r_ = rd[gi % 2]
                rn_ = 'c_rd%d' % (gi % 2)
                o_ = oc[gi % 2]
                on_ = 'c_oc%d' % (gi % 2)
                kb.recip(r_[:, :n], pD[:, :n], pDn, [rn_])
                kb.tt('dve', o_[:, :n], pO[:, :n], r_[:, :n], ALU.mult, pOn + [rn_], [on_])
                kb.dma(mixT[256:384, q0:q0 + n], o_[:, :n], [on_], ['mixT_c%d' % gi])
            kb.end_phase(upto == 3)
            if upto == 3:
                return nc

        with kb.phase():
            ksc = 128 ** -0.5
            cst = kb.sb("b_cst", [128, 16], F32)
            kb.act(cst[:, 0:2], smt[:, 1:3], AF.Exp, ['smt'], ['b_cst'], scale=-math.log(2.0))
            kb.ts('dve', cst[:, 0:2], cst[:, 0:2], -1.0, ALU.mult, ['b_cst'], ['b_cst'], s2=1.0, op1=ALU.add)
            kb.act(cst[:, 0:2], cst[:, 0:2], AF.Ln, ['b_cst'], ['b_cst'])
            kb.ts('dve', cst[:, 2:3], cst[:, 1:2], -1.0, ALU.mult, ['b_cst'], ['b_cst'])
            kb.act(cst[:, 3:5], cst[:, 0:2], AF.Exp, ['b_cst'], ['b_cst'], scale=128.0)
            io = kb.sb("b_io", [128, 128], F32)
            DT = kb.sb("b_DT", [128, 128], F32)
            DT2 = kb.sb("b_DT2", [128, 128], F32)
            GF = kb.sb("b_GF", [128, 128], F32)
            GB = kb.sb("b_GB", [128, 128], F32)
            pcol = kb.sb("b_pcol", [128, 2], F32)
            kb.iota(io[:], [[1, 128]], 0, -1, ['b_io'])
            kb.act(DT[:], io[:], AF.Exp, ['b_io', 'b_cst'], ['b_DT'], scale=cst[:, 0:1])
            kb.asel(DT[:], DT[:], [[1, 128]], ALU.is_ge, 0.0, 0, -1, ['b_DT'], ['b_DT'])
            kb.act(DT2[:], io[:], AF.Exp, ['b_io', 'b_cst'], ['b_DT2'], scale=cst[:, 2:3])
            kb.asel(DT2[:], DT2[:], [[-1, 128]], ALU.is_ge, 0.0, 0, 1, ['b_DT2'], ['b_DT2'])
            kb.tt('dve', DT[:], DT[:], DT2[:], ALU.add, ['b_DT', 'b_DT2'], ['b_DT'])
            kb.ts('dve', DT[:], DT[:], ksc, ALU.mult, ['b_DT'], ['b_DT'])
            kb.iota(io[:], [[1, 128]], 1, 0, ['b_io'])
            kb.act(GF[:], io[:], AF.Exp, ['b_io', 'b_cst'], ['b_GF'], scale=cst[:, 0:1])
            kb.iota(io[:], [[-1, 128]], 128, 0, ['b_io'])
            kb.act(GB[:], io[:], AF.Exp, ['b_io', 'b_cst'], ['b_GB'], scale=cst[:, 1:2])
            kb.iota(pcol[:, 0:1], [[0, 1]], 127, -1, ['b_pcol'])
            kb.iota(pcol[:, 1:2], [[0, 1]], 0, 1, ['b_pcol'])
            kb.act(cst[:, 5:6], pcol[:, 0:1], AF.Exp, ['b_pcol', 'b_cst'], ['b_cst'], scale=cst[:, 0:1])
            kb.act(cst[:, 6:7], pcol[:, 1:2], AF.Exp, ['b_pcol', 'b_cst'], ['b_cst'], scale=cst[:, 1:2])
            kb.ts('dve', cst[:, 5:7], cst[:, 5:7], ksc, ALU.mult, ['b_cst'], ['b_cst'])
            if upto == 3.1:
                kb.end_phase(True)
                return nc
            qf32 = kb.sb("b_qf32", [128, NTOK], F32)
            tmpf = kb.sb("b_tmpf", [128, NTILE, 128], F32)
            rqb = kb.sb("b_rqb", [128, NTOK], BF16)
            rkb = kb.sb("b_rkb", [128, NTOK], BF16)
            QfT = kb.sb("b_QfT", [128, NTILE, 128], BF16)
            QbT = kb.sb("b_QbT", [128, NTILE, 128], BF16)
            rvb = kb.sb("b_rvb", [128, NTILE, 128], BF16)
            Kst = [kb.sb("b_Kst%d" % d, [128, NTILE, 128], BF16) for d in range(2)]
            hist = [kb.sb("b_hist%d" % d, [128, NTILE, 128], BF16) for d in range(2)]
            Sst = [[kb.sb("b_S%d_%d" % (d, i), [128, 128], F32) for i in range(2)] for d in range(2)]
            oB = kb.sb("b_o", [128, NTOK], F32)
            load_fm(qf32, "rq", wname='b_qf32')
            kb.cp('act', rqb[:], qf32[:], ['b_qf32'], ['b_rqb'])
            q3 = qf32[:].rearrange("p (t n) -> p t n", n=128)
            kb.tt('dve', QfT[:], q3, GF[:].unsqueeze(1).to_broadcast([128, NTILE, 128]), ALU.mult,
                  ['b_qf32', 'b_GF'], ['b_QfT'])
            kb.tt('pool', QbT[:], q3, GB[:].unsqueeze(1).to_broadcast([128, NTILE, 128]), ALU.mult,
                  ['b_qf32', 'b_GB'], ['b_QbT'])
            load_fm(qf32, "rk", wname='b_qf32')
            kb.cp('act', rkb[:], qf32[:], ['b_qf32'], ['b_rkb'])
            load_tm(tmpf, "rv", 'b_tmpf')
            kb.cp('dve', rvb[:], tmpf[:], ['b_tmpf'], ['b_rvb'])
            load_tm(tmpf, "rk", 'b_tmpf')
            kb.ts('dve', Kst[0][:], tmpf[:], cst[:, 5:6], ALU.mult, ['b_tmpf', 'b_cst'], ['b_Kst0'])
            kb.ts('pool', Kst[1][:], tmpf[:], cst[:, 6:7], ALU.mult, ['b_tmpf', 'b_cst'], ['b_Kst1'])
            if upto == 3.2:
                kb.end_phase(True)
                return nc
            orders = [list(range(NTILE)), [1, 0] + list(range(NTILE - 1, 1, -1))]
            for d in range(2):
                kb.memset('pool', Sst[d][0][:], 0.0, ['b_S%d_0' % d])
                for i, t in enumerate(orders[d]):
                    cur, nxt = Sst[d][i % 2], Sst[d][(i + 1) % 2]
                    cn_, nn_ = 'b_S%d_%d' % (d, i % 2), 'b_S%d_%d' % (d, (i + 1) % 2)
                    kb.cp('act', hist[d][:, t, :], cur[:], [cn_], ['b_hist%d_%d' % (d, t)])
                    pS, pSn = kb.pslot((0, 1, 2))
                    kb.mm(pS, Kst[d][:, t, :], rvb[:, t, :], True, True, ['b_Kst%d' % d, 'b_rvb'], pSn)
                    kb.stt('dve', nxt[:], cur[:], cst[:, 3 + d:4 + d], pS, ALU.mult, ALU.add,
                           [cn_, 'b_cst'] + pSn, [nn_])
            sdt = [kb.sb("b_sdt%d" % i, [128, 128], BF16) for i in range(3)]
            if upto == 3.3:
                kb.end_phase(True)
                return nc
            for t in range(NTILE):
                pS, pSn = kb.pslot((3, 4))
                kb.mm(pS, rkb[:, t * 128:(t + 1) * 128], rqb[:, t * 128:(t + 1) * 128], True, True,
                      ['b_rkb', 'b_rqb'], pSn)
                s_ = sdt[t % 3]
                sn_ = 'b_sdt%d' % (t % 3)
                kb.tt('dve', s_[:], pS, DT[:], ALU.mult, pSn + ['b_DT'], [sn_])
                pO, pOn = kb.pslot((5, 0, 1))
                kb.mm(pO, rvb[:, t, :], s_[:], True, False, ['b_rvb', sn_], pOn)
                kb.mm(pO, hist[0][:, t, :], QfT[:, t, :], False, False, ['b_hist0_%d' % t, 'b_QfT'], pOn)
                kb.mm(pO, hist[1][:, t, :], QbT[:, t, :], False, True, ['b_hist1_%d' % t, 'b_QbT'], pOn)
                kb.cp('act', oB[:, t * 128:(t + 1) * 128], pO, pOn, ['b_o'])
            gate = kb.sb("b_gate", [128, NTOK], F32)
            load_fm(gate, "rg", wname='b_gate')
            kb.act(gate[:], gate[:], AF.Silu, ['b_gate'], ['b_gate'])
            ob1 = kb.sb("b_ob1", [128, 512], BF16)
            ob2 = kb.sb("b_ob2", [128, 512], BF16)
            mean = kb.sb("b_mean", [128, 512], F32)
            var = kb.sb("b_var", [128, 512], F32)
            yo = [kb.sb("b_yo%d" % i, [128, 512], F32) for i in range(2)]
            for g, (g0, n) in enumerate(GROUPS):
                kb.cp('act', ob1[:, :n], oB[:, g0:g0 + n], ['b_o'], ['b_ob1'])
                kb.tt('pool', ob2[:, :n], oB[:, g0:g0 + n], oB[:, g0:g0 + n], ALU.mult, ['b_o'], ['b_ob2'])
                pM, pMn = kb.pbank((0, 1))
                pV, pVn = kb.pbank((2, 3))
                kb.mm(pM[:, :n], ones_b[:], ob1[:, :n], True, True, ['ones_b', 'b_ob1'], pMn)
                kb.mm(pV[:, :n], ones_b[:], ob2[:, :n], True, True, ['ones_b', 'b_ob2'], pVn)
                kb.ts('dve', mean[:, :n], pM[:, :n], 1.0 / 128, ALU.mult, pMn, ['b_mean'])
                kb.tt('dve', var[:, :n], mean[:, :n], mean[:, :n], ALU.mult, ['b_mean'], ['b_var'])
                kb.stt('dve', var[:, :n], pV[:, :n], 1.0 / 128, var[:, :n], ALU.mult, ALU.subtract, pVn + ['b_var'], ['b_var'])
                kb.rsqrt(var[:, :n], var[:, :n], 1.0, ['b_var'], ['b_var'])
                y_ = yo[g % 2]
                yn_ = 'b_yo%d' % (g % 2)
                kb.tt('dve', y_[:, :n], oB[:, g0:g0 + n], mean[:, :n], ALU.subtract, ['b_o', 'b_mean'], [yn_])
                kb.stt('dve', y_[:, :n], y_[:, :n], smt[:, 3:4], var[:, :n], ALU.mult, ALU.mult, [yn_, 'smt', 'b_var'], [yn_])
                kb.tt('pool', y_[:, :n], y_[:, :n], gate[:, g0:g0 + n], ALU.mult, [yn_, 'b_gate'], [yn_])
                kb.dma(mixT[128:256, g0:g0 + n], y_[:, :n], [yn_], ['mixT_b%d' % g])
            kb.end_phase(upto == 4)
            if upto == 4:
                return nc

        with kb.phase():
            NCH = NTOK // 16
            lbc = kb.sb("d_lbc", [128, 4], F32)
            kb.tt('dve', lbc[:, 0:2], smt[:, 11:13], smt[:, 9:11], ALU.subtract, ['smt'], ['d_lbc'])
            kb.act(lbc[:, 0:2], lbc[:, 0:2], AF.Sigmoid, ['d_lbc'], ['d_lbc'])
            kb.ts('dve', lbc[:, 0:2], lbc[:, 0:2], smt[:, 13:14], ALU.mult, ['d_lbc', 'smt'], ['d_lbc'])
            kb.ts('dve', lbc[:, 2:4], lbc[:, 0:2], -1.0, ALU.mult, ['d_lbc'], ['d_lbc'], s2=1.0, op1=ALU.add)
            mt = kb.sb("d_mt", [128, 128], F32)
            MT = [kb.sb("d_MT%d" % d, [128, 128], BF16) for d in range(2)]
            RM = kb.sb("d_RM", [128, 8], F32)
            RMb = kb.sb("d_RMb", [128, 8], BF16)
            for d in range(2):
                kb.memset('pool', mt[:], 1.0, ['d_mt'])
                m3 = mt[:].rearrange("p (c i) -> p c i", i=16)
                kb.asel(m3, m3, [[-16, 8], [0, 16]], ALU.is_ge, 0.0, 0, 1, ['d_mt'], ['d_mt'])
                kb.asel(m3, m3, [[16, 8], [0, 16]], ALU.is_ge, 0.0, 15, -1, ['d_mt'], ['d_mt'])
                if d == 0:
                    kb.asel(mt[:], mt[:], [[1, 128]], ALU.is_ge, 0.0, 0, -1, ['d_mt'], ['d_mt'])
                else:
                    kb.asel(mt[:], mt[:], [[-1, 128]], ALU.is_ge, 0.0, 0, 1, ['d_mt'], ['d_mt'])
                kb.cp('dve', MT[d][:], mt[:], ['d_mt'], ['d_MT%d' % d])
            kb.memset('pool', RM[:], 1.0, ['d_RM'])
            kb.asel(RM[:], RM[:], [[-16, 8]], ALU.is_ge, 0.0, 0, 1, ['d_RM'], ['d_RM'])
            kb.asel(RM[:], RM[:], [[16, 8]], ALU.is_ge, 0.0, 15, -1, ['d_RM'], ['d_RM'])
            kb.cp('dve', RMb[:], RM[:], ['d_RM'], ['d_RMb'])
            qs = kb.sb("d_qs", [128, NTOK], F32)
            zk = kb.sb("d_zk", [128, NTOK], F32)
            G1 = kb.sb("d_G1", [128, NTOK], F32)
            G2 = kb.sb("d_G2", [128, NTOK], F32)
            Qin = [kb.sb("d_Qin%d" % d, [128, NTOK], BF16) for d in range(2)]
            Kd = [kb.sb("d_Kd%d" % d, [128, NTOK], BF16) for d in range(2)]
            KstT = kb.sb("d_KstT", [128, NTOK], BF16)
            Kst = [kb.sb("d_Kst%d" % d, [128, NTILE, 128], BF16) for d in range(2)]
            alast = [kb.sb("d_al%d" % d, [128, NCH], F32) for d in range(2)]
            Vb = kb.sb("d_Vb", [128, NTILE, 128], BF16)
            oD = kb.sb("d_o", [128, NTOK], F32)
            load_fm(qs, "hq", wname='d_qs')
            kb.act(qs[:], qs[:], AF.Silu, ['d_qs'], ['d_qs'])
            for d in range(2):
                load_fm(zk, "hff" if d == 0 else "hfb", wname='d_zk')
                kb.act(zk[:], zk[:], AF.Sigmoid, ['d_zk'], ['d_zk'])
                kb.ts('dve', zk[:], zk[:], lbc[:, 2 + d:3 + d], ALU.mult, ['d_zk', 'd_lbc'], ['d_zk'],
                      s2=lbc[:, d:d + 1], op1=ALU.add)
                kb.act(G1[:], zk[:], AF.Ln, ['d_zk'], ['d_G1'])
                kb.ts('dve', zk[:], zk[:], -1.0, ALU.mult, ['d_zk'], ['d_zk'], s2=1.0, op1=ALU.add)
                src, dst = G1, G2
                sn, dn_ = 'd_G1', 'd_G2'
                for s in (1, 2, 4, 8):
                    s3 = src[:].rearrange("p (c i) -> p c i", i=16)
                    d3 = dst[:].rearrange("p (c i) -> p c i", i=16)
                    if d == 0:
                        kb.tt('dve', d3[:, :, s:], s3[:, :, s:], s3[:, :, :16 - s], ALU.add, [sn], [dn_])
                        kb.cp('pool', d3[:, :, :s], s3[:, :, :s], [sn], [dn_])
                    else:
                        kb.tt('dve', d3[:, :, :16 - s], s3[:, :, :16 - s], s3[:, :, s:], ALU.add, [sn], [dn_])
                        kb.cp('pool', d3[:, :, 16 - s:], s3[:, :, 16 - s:], [sn], [dn_])
                    src, dst = dst, src
                    sn, dn_ = dn_, sn
                b3 = src[:].rearrange("p (c i) -> p c i", i=16)
                e3 = dst[:].rearrange("p (c i) -> p c i", i=16)
                bl = b3[:, :, 15] if d == 0 else b3[:, :, 0]
                kb.act(alast[d][:], bl, AF.Exp, [sn], ['d_al%d' % d])
                kb.act(dst[:], src[:], AF.Exp, [sn], [dn_])
                kb.tt('dve', Qin[d][:], qs[:], dst[:], ALU.mult, ['d_qs', dn_], ['d_Qin%d' % d])
                kb.act(dst[:], src[:], AF.Exp, [sn], [dn_], scale=-1.0)
                kb.tt('dve', Kd[d][:], zk[:], dst[:], ALU.mult, ['d_zk', dn_], ['d_Kd%d' % d])
                kb.tt('dve', e3, bl.unsqueeze(2).to_broadcast([128, NCH, 16]), b3, ALU.subtract, [sn], [dn_])
                kb.act(dst[:], dst[:], AF.Exp, [dn_], [dn_])
                kb.tt('dve', KstT[:], zk[:], dst[:], ALU.mult, ['d_zk', dn_], ['d_KstT'])
                for t in range(NTILE):
                    pbb = kb.psb[t % 2]
                    pbbn = ['psb%d' % (t % 2)]
                    kb.tr(pbb[:, 0:128], KstT[:, t * 128:(t + 1) * 128], ident_b[:], ['d_KstT', 'ident_b'], pbbn)
                    kb.cp('act' if t % 2 == 0 else 'dve', Kst[d][:, t, :], pbb[:, 0:128], pbbn, ['d_Kst%d' % d])
            vtmp = G1
            load_tm(vtmp[:].rearrange("p (t c) -> p t c", c=128), "hi", 'd_G1')
            kb.cp('act', Vb[:], vtmp[:].rearrange("p (t c) -> p t c", c=128), ['d_G1'], ['d_Vb'])
            Vm = [kb.sb("d_Vm%d" % i, [128, 8, 128], BF16) for i in range(2)]
            H3 = [kb.sb("d_H_%d" % i, [128, 9, 128], F32) for i in range(3)]
            Hb = [kb.sb("d_Hb%d" % i, [128, 8, 128], BF16) for i in range(2)]
            STb = [kb.sb("d_ST%d" % i, [128, 128], BF16) for i in range(4)]
            orders = [list(range(NTILE)), [1, 0] + list(range(NTILE - 1, 1, -1))]
            vm_rr = 0
            st_rr = 0
            for d in (1, 0):
                kb.memset('pool', H3[0][:, 0, :], 0.0, ['d_H_0'])
                for it, t in enumerate(orders[d]):
                    Hc, Hn = H3[it % 3], H3[(it + 1) % 3]
                    hcn, hnn = 'd_H_%d' % (it % 3), 'd_H_%d' % ((it + 1) % 3)
                    vm = Vm[vm_rr % 2]
                    vmn = 'd_Vm%d' % (vm_rr % 2)
                    vm_rr += 1
                    kb.tt('pool', vm[:], Vb[:, t, :].unsqueeze(1).to_broadcast([128, 8, 128]),
                          RMb[:].unsqueeze(2).to_broadcast([128, 8, 128]), ALU.mult, ['d_Vb', 'd_RMb'], [vmn])
                    for i in range(8):
                        j = i if d == 0 else 7 - i
                        c = t * 8 + j
                        pS, pSn = kb.pslot((0, 1, 2))
                        kb.mm(pS, Kst[d][:, t, :], vm[:, j, :], True, True, ['d_Kst%d' % d, vmn], pSn)
                        if i < 7:
                            kb.stt('dve', Hc[:, i + 1, :], Hc[:, i, :], alast[d][:, c:c + 1], pS, ALU.mult, ALU.add,
                                   [hcn, 'd_al%d' % d] + pSn, [hcn])
                        else:
                            kb.stt('dve', Hn[:, 0, :], Hc[:, i, :], alast[d][:, c:c + 1], pS, ALU.mult, ALU.add,
                                   [hcn, 'd_al%d' % d] + pSn, [hnn])
                    hb = Hb[it % 2]
                    hbn = 'd_Hb%d' % (it % 2)
                    kb.cp('act', hb[:], Hc[:, 0:8, :], [hcn], [hbn])
                    pI, pIn = kb.pslot((3, 4))
                    for i in range(8):
                        j = i if d == 0 else 7 - i
                        kb.mm(pI[:, j * 16:(j + 1) * 16], hb[:, i, :], Qin[d][:, t * 128 + j * 16:t * 128 + (j + 1) * 16],
                              True, True, [hbn, 'd_Qin%d' % d], pIn)
                    if d == 1:
                        kb.cp('act', oD[:, t * 128:(t + 1) * 128], pI, pIn, ['d_o%d' % t])
                    else:
                        kb.tt('dve', oD[:, t * 128:(t + 1) * 128], oD[:, t * 128:(t + 1) * 128], pI, ALU.add,
                              ['d_o%d' % t] + pIn, ['d_o%d' % t])
                        pX, pXn = kb.pslot((5,))
                        sts = []
                        for dd in range(2):
                            pS, pSn = kb.pslot((0, 1, 2))
                            kb.mm(pS, Kd[dd][:, t * 128:(t + 1) * 128], Qin[dd][:, t * 128:(t + 1) * 128], True, True,
                                  ['d_Kd%d' % dd, 'd_Qin%d' % dd], pSn)
                            s_ = STb[st_rr % 4]
                            sn_ = 'd_ST%d' % (st_rr % 4)
                            st_rr += 1
                            kb.tt('dve', s_[:], pS, MT[dd][:], ALU.mult, pSn + ['d_MT%d' % dd], [sn_])
                            sts.append((s_, sn_))
                        for dd in range(2):
                            kb.mm(pX, Vb[:, t, :], sts[dd][0][:], dd == 0, dd == 1, ['d_Vb', sts[dd][1]], pXn)
                        kb.tt('dve', oD[:, t * 128:(t + 1) * 128], oD[:, t * 128:(t + 1) * 128], pX, ALU.add,
                              ['d_o%d' % t] + pXn, ['d_o%d' % t])
            o_r = ['d_o%d' % t for t in range(NTILE)]
            gate = zk
            load_fm(gate, "hg", wname='d_zk')
            kb.act(gate[:], gate[:], AF.Silu, ['d_zk'], ['d_zk'])
            ob2 = kb.sb("d_ob2", [128, 512], BF16)
            var = kb.sb("d_var", [128, 512], F32)
            yo = [kb.sb("d_yo%d" % i, [128, 512], F32) for i in range(2)]
            for g, (g0, n) in enumerate(GROUPS):
                kb.tt('pool', ob2[:, :n], oD[:, g0:g0 + n], oD[:, g0:g0 + n], ALU.mult, o_r, ['d_ob2'])
                pV, pVn = kb.pbank((0, 1))
                kb.mm(pV[:, :n], ones_b[:], ob2[:, :n], True, True, ['ones_b', 'd_ob2'], pVn)
                kb.rsqrt(var[:, :n], pV[:, :n], 1.0 / 128, pVn, ['d_var'])
                y_ = yo[g % 2]
                yn_ = 'd_yo%d' % (g % 2)
                kb.stt('dve', y_[:, :n], oD[:, g0:g0 + n], smt[:, 8:9], var[:, :n], ALU.mult, ALU.mult,
                       o_r + ['smt', 'd_var'], [yn_])
                kb.tt('pool', y_[:, :n], y_[:, :n], gate[:, g0:g0 + n], ALU.mult, [yn_, 'd_zk'], [yn_])
                kb.dma(mixT[384:512, g0:g0 + n], y_[:, :n], [yn_], ['mixT_d%d' % g])
            P.finish()
            P.emit()
    return nc


def _rope_tables():
    rows = T // 64
    row = np.repeat(np.arange(rows), 64).astype(np.float32)
    col = np.tile(np.arange(64), rows).astype(np.float32)

    def ang(rot_dim):
        nf = rot_dim // 4
        inv = (np.float32(10000.0) ** (-np.arange(nf, dtype=np.float32) / np.float32(nf))).astype(np.float32)
        return np.concatenate([row[:, None] * inv, col[:, None] * inv], -1).astype(np.float32)

    def tabs(a):
        half = a.shape[1]
        c = np.cos(a).T.astype(np.float32)
        s = np.sin(a).T.astype(np.float32)
        C2 = np.ones((2 * half, NTOK), np.float32)
        S2 = np.zeros((2 * half, NTOK), np.float32)
        C2[:half, L:] = c
        C2[half:, L:] = c
        S2[:half, L:] = -s
        S2[half:, L:] = s
        return C2, S2
    return tabs(ang(128)) + tabs(ang(64))


def _win_cols(h):
    kv = h // 2
    offs = np.cumsum([0, 512, 256, 256, 512, 512, 512, 512, 384, 128, 64, 512, 512, 512, 512, 512])
    (o_sq, o_sk, o_sv, o_rq, o_rk, o_rv, o_rg, o_cq, o_ckv, o_kr, o_hq, o_hff, o_hfb, o_hi, o_hg) = offs[:15]
    r = lambda o, i, w=128: list(range(o + i * w, o + (i + 1) * w))
    cols = (r(o_sq, h) + r(o_sk, kv) + r(o_sv, kv) + r(o_rq, h) + r(o_rk, h) + r(o_rv, h) + r(o_rg, h)
            + list(range(o_cq, o_cq + 384)) + list(range(o_ckv, o_ckv + 128)) + list(range(o_kr, o_kr + 64))
            + r(o_hq, h) + r(o_hff, h) + r(o_hfb, h) + r(o_hi, h) + r(o_hg, h))
    assert len(cols) == NWIN
    return np.array(cols)


def prep_A(inp, l, b, h, x_cur, ctx_cur, tabs):
    f = lambda a: np.ascontiguousarray(a, dtype=np.float32)
    cvec = np.stack([inp["c"][b], inp["c_ctx"]], 0)
    cT = cvec.reshape(2, 16, 128).transpose(2, 1, 0).reshape(128, 32)
    badaT = inp["b_ada"][l][:4096].reshape(32, 128).T
    sm = np.zeros((128, 16), np.float32)
    sm[:, 0] = inp["swa_sink"][l][h]
    sm[:, 1] = inp["ret_decay_exp"][l][0, h]
    sm[:, 2] = inp["ret_decay_exp"][l][1, h]
    sm[:, 3] = inp["ret_gn"][l][h * 128:(h + 1) * 128]
    sm[:, 4] = inp["mla_kv_norm"][l]
    sm[:, 5:8] = inp["mla_q_norm"][l].reshape(3, 128).T
    sm[:, 8] = inp["hgrn_gn"][l][h * 128:(h + 1) * 128]
    lg = inp["hgrn_lb_logits"][:, :, h * 128:(h + 1) * 128]
    sm[:, 9] = lg[0, 0]
    sm[:, 10] = lg[0, 1]
    sm[:, 11] = lg[1, 0]
    sm[:, 12] = lg[1, 1]
    sm[:, 13] = 0.0 if l == 0 else 1.0
    wq = inp["mla_w_uq"][l][:, h * 192:(h + 1) * 192]
    wuq = np.concatenate([wq, wq[:, 160:192], wq[:, 128:160]], 1)
    return {
        "xin": f(np.concatenate([ctx_cur[b], x_cur[b]], 0)),
        "cT": f(cT), "wada": f(inp["w_ada"][l][:, :4096]), "badaT": f(badaT),
        "win": f(inp["w_in"][l][:, _win_cols(h)]),
        "ropeAc": tabs[0], "ropeAs": tabs[1], "ropeMc": tabs[2], "ropeMs": tabs[3],
        "sm": sm, "wuq": f(wuq), "wukv": f(inp["mla_w_ukv"][l][:, h * 256:(h + 1) * 256]),
    }


NB = 1280
NBT = 10
BIG = 1.0e30


def build_B(upto=99):
    nc = bass.Bass("TRN2", target_bir_lowering=False)
    dt_in = lambda n, s: nc.dram_tensor(n, s, F32, kind="ExternalInput").ap()
    mixq = dt_in("mixq", [D, NB])
    xin = dt_in("xin", [NB, D])
    cT = dt_in("cT", [128, 32])
    wada = dt_in("wada", [D, 8192])
    badaT = dt_in("badaT", [128, 64])
    wout = dt_in("wout", [D, D])
    lnv = dt_in("lnv", [4, D])
    wr = dt_in("wr", [D, 36])
    w1 = dt_in("w1", [32, D, 512])
    w3 = dt_in("w3", [32, D, 512])
    w2 = dt_in("w2", [32, 512, D])
    outp = nc.dram_tensor("outp", [NB, D], F32, kind="ExternalOutput").ap()
    x1s = nc.dram_tensor("x1s", [NB, D], F32).ap()
    h2s = nc.dram_tensor("h2s", [NBT, 128, 16, 128], BF16).ap()

    with ExitStack() as es:
        kb = KB(nc, es)
        P = kb.P
        gsb = lambda n, s, d: es.enter_context(nc.sbuf_tensor(n, s, d))
        ones_f = gsb("ones_f", [128, 128], F32)
        ident_f = gsb("ident_f", [128, 128], F32)
        modB = gsb("modB", [128, 64, 2], F32)
        Gt = gsb("Gt", [128, NBT, 32], F32)
        kb.memset('pool', ones_f[:], 1.0, ['ones_f'])
        kb.memset('pool', ident_f[:], 1.0, ['ident_f'])
        kb.asel(ident_f[:], ident_f[:], [[-1, 128]], ALU.is_equal, 0.0, 0, 1, ['ident_f'], ['ident_f'])

        def make_bc(dst, base, r, nm):
            dg = kb.sb("bc_dg_" + nm, [128, 128], F32)
            for k in range(16):
                kb.ts('dve', dg[:], ident_f[:], modB[:, base + k, r:r + 1], ALU.mult, ['ident_f', 'modB'], ['bc_dg' + nm])
                pS, pSn = kb.pslot((0, 1, 2, 3))
                kb.mm(pS, ones_f[:], dg[:], True, True, ['ones_f', 'bc_dg' + nm], pSn)
                kb.cp('act', dst[:, k * 128:(k + 1) * 128], pS, pSn, [nm])

        with kb.phase():
            cTs = kb.sb("cTs", [128, 32], F32)
            sT = kb.sb("sT", [128, 32], F32)
            bT = kb.sb("bT", [128, 64], F32)
            wk = [kb.sb("wk%d" % i, [128, 8192], F32) for i in range(2)]
            kb.dma(cTs[:], cT[:, :], (), ['cTs'])
            kb.dma(bT[:], badaT[:, :], (), ['bT'])
            kb.act(sT[:], cTs[:], AF.Silu, ['cTs'], ['sT'])
            pb = kb.psf[0]
            pbn = ["ps0_%d" % s_ for s_ in range(4)]
            for k in range(16):
                w = wk[k % 2]
                wn = 'wk%d' % (k % 2)
                kb.dma(w[:, 0:4096], wada[k * 128:(k + 1) * 128, 0:4096], (), [wn], q='sp')
                kb.dma(w[:, 4096:8192], wada[k * 128:(k + 1) * 128, 4096:8192], (), [wn], q='act')
                for j in range(64):
                    kb.mm(pb[:, 2 * j:2 * j + 2], w[:, j * 128:(j + 1) * 128], sT[:, 2 * k:2 * k + 2],
                          k == 0 and j == 0, k == 15, [wn, 'sT'], pbn)
            kb.tt('dve', modB[:], pb[:, 0:128].rearrange("p (j r) -> p j r", r=2),
                  bT[:].unsqueeze(2).to_broadcast([128, 64, 2]), ALU.add, pbn + ['bT'], ['modB'])
            kb.ts('dve', modB[:, 32:48, :], modB[:, 32:48, :], 1.0, ALU.add, ['modB'], ['modB'])
            kb.end_phase(upto == 0)
            if upto == 0:
                return nc

        with kb.phase():
            woutb = kb.sb("woutb", [128, 16, D], BF16)
            for k in range(16):
                kb.dma(woutb[:, k, :], wout[k * 128:(k + 1) * 128, :], (), ['woutb%d' % k], q='pool')
            wo_r = ['woutb%d' % k for k in range(16)]
            lnb = [kb.sb("lnb%d" % i, [128, D], F32) for i in range(2)]
            for i in range(2):
                kb.dma(lnb[i][:], lnv[i, :].partition_broadcast(128), (), ['lnb%d' % i])
            G1 = [kb.sb("G1_%d" % r, [128, D], F32) for r in range(2)]
            for r in range(2):
                make_bc(G1[r], 0, r, 'G1_%d' % r)
            wrs = kb.sb("wrs", [128, 16, 36], F32)
            kb.dma(wrs[:], wr.rearrange("(k p) n -> p k n", p=128), (), ['wrs'])
            mixb = [kb.sb("mixb%d" % i, [128, 16, 128], BF16) for i in range(2)]
            xt = [kb.sb("xt%d" % i, [128, D], F32) for i in range(2)]
            rb = kb.sb("rb", [128, D], F32)
            x1t = kb.sb("x1t", [128, D], F32)
            xn2 = kb.sb("xn2", [128, D], F32)
            h2f = kb.sb("h2f", [128, 16, 128], F32)
            h2b = kb.sb("h2b", [128, 16, 128], BF16)
            stats = kb.sb("stats", [128, 4, 6], F32)
            mv = kb.sb("mv", [128, 4], F32)
            rt = kb.sb("rt", [128, 128], F32)
            for t in range(NBT):
                r = 1 if t < 2 else 0
                mb = mixb[t % 2]
                mbn = 'mixb%d' % (t % 2)
                kb.dma(mb[:], mixq[:, t * 128:(t + 1) * 128].rearrange("(k p) n -> p k n", p=128), (), [mbn], q='pool')
                x_ = xt[t % 2]
                xn_ = 'xt%d' % (t % 2)
                kb.dma(x_[:], xin[t * 128:(t + 1) * 128, :], (), [xn_])
                for cb in range(4):
                    pb, pbn = kb.pbank((0, 1, 2, 3))
                    for k in range(16):
                        kb.mm(pb[:, :], mb[:, k, :], woutb[:, k, cb * 512:(cb + 1) * 512], k == 0, k == 15,
                              [mbn, wo_r[k]], pbn)
                    kb.tt('dve', rb[:, cb * 512:(cb + 1) * 512], pb[:, :], G1[r][:, cb * 512:(cb + 1) * 512], ALU.mult,
                          pbn + ['G1_%d' % r], ['rb'])
                kb.stt('dve', rb[:], x_[:], ALPHA, rb[:], ALU.mult, ALU.add, [xn_, 'rb'], ['rb'])
                for c in range(4):
                    P.op('dve', lambda e, c=c: e.bn_stats(out=stats[:, c, :], in_=rb[:, c * 512:(c + 1) * 512]), ['rb'], ['stats'])
                P.op('dve', lambda e: e.bn_aggr(out=mv[:, 0:2], in_=stats[:]), ['stats'], ['mv'])
                kb.rsqrt(mv[:, 1:2], mv[:, 1:2], 1.0, ['mv'], ['mv'])
                kb.ts('dve', mv[:, 2:3], mv[:, 0:1], mv[:, 1:2], ALU.mult, ['mv'], ['mv'], s2=-1.0, op1=ALU.mult)
                kb.act(x1t[:], rb[:], AF.Identity, ['rb', 'mv'], ['x1t'], bias=mv[:, 2:3], scale=mv[:, 1:2])
                kb.tt('dve', x1t[:], x1t[:], lnb[0][:], ALU.mult, ['x1t', 'lnb0'], ['x1t'])
                kb.tt('pool', x1t[:], x1t[:], lnb[1][:], ALU.add, ['x1t', 'lnb1'], ['x1t'])
                kb.dma(x1s[t * 128:(t + 1) * 128, :], x1t[:], ['x1t'], ['x1s%d' % t])
                for c in range(4):
                    P.op('dve', lambda e, c=c: e.bn_stats(out=stats[:, c, :], in_=x1t[:, c * 512:(c + 1) * 512]), ['x1t'], ['stats'])
                P.op('dve', lambda e: e.bn_aggr(out=mv[:, 0:2], in_=stats[:]), ['stats'], ['mv'])
                kb.rsqrt(mv[:, 1:2], mv[:, 1:2], 1.0, ['mv'], ['mv'])
                kb.ts('dve', mv[:, 2:3], mv[:, 0:1], mv[:, 1:2], ALU.mult, ['mv'], ['mv'], s2=-1.0, op1=ALU.mult)
                kb.act(xn2[:], x1t[:], AF.Identity, ['x1t', 'mv'], ['xn2'], bias=mv[:, 2:3], scale=mv[:, 1:2])
                for q4 in range(4):
                    pb, pbn = kb.pbank((4, 5))
                    for kk in range(4):
                        k = q4 * 4 + kk
                        kb.tr(pb[:, kk * 128:(kk + 1) * 128], xn2[:, k * 128:(k + 1) * 128], ident_f[:], ['xn2', 'ident_f'], pbn)
                    for kk in range(4):
                        k = q4 * 4 + kk
                        if kk % 2 == 0:
                            kb.ts('dve', h2f[:, k, :], pb[:, kk * 128:(kk + 1) * 128], modB[:, 32 + k, r:r + 1], ALU.mult,
                                  pbn + ['modB'], ['h2f'], s2=modB[:, 16 + k, r:r + 1], op1=ALU.add)
                        else:
                            kb.act(h2f[:, k, :], pb[:, kk * 128:(kk + 1) * 128], AF.Identity, pbn + ['modB'], ['h2f'],
                                   bias=modB[:, 16 + k, r:r + 1], scale=modB[:, 32 + k, r:r + 1])
                kb.cp('pool', h2b[:], h2f[:], ['h2f'], ['h2b'])
                kb.dma(h2s[t], h2b[:], ['h2b'], ['h2s%d' % t])
                pb, pbn = kb.pbank((0, 1, 2, 3))
                for k in range(16):
                    kb.mm(pb[:, 0:36], h2f[:, k, :], wrs[:, k, :], k == 0, k == 15, ['h2f', 'wrs'], pbn)
                lg = rt[:, 0:36]
                kb.cp('act', lg, pb[:, 0:36], pbn, ['rt'])
                R = ['rt']
                gmax, ngmax, sg, l1, nl1, l2, e2, w1g, w2g = [rt[:, 40 + i:41 + i] for i in range(9)]
                Mg, eg, pen = rt[:, 52:56], rt[:, 56:60], rt[:, 60:64]
                lm, m1, lm2 = rt[:, 64:96], rt[:, 96:128], None
                kb.rmax(gmax, lg[:, 0:4], R, R)
                kb.ts('dve', ngmax, gmax, -1.0, ALU.mult, R, R)
                kb.ts('dve', Mg, lg[:, 0:4], gmax, ALU.is_equal, R, R)
                kb.act(eg, lg[:, 0:4], AF.Exp, R, R, bias=ngmax, scale=1.0)
                P.op('dve', lambda e: e.reduce_sum(out=sg, in_=eg, axis=AX.X), R, R)
                kb.recip(sg, sg, R, R)
                kb.ts('dve', pen, Mg, -1.0, ALU.add, R, R, s2=BIG, op1=ALU.mult)
                kb.tt('dve', lm.rearrange("p (g j) -> p g j", j=8), lg[:, 4:36].rearrange("p (g j) -> p g j", j=8),
                      pen.unsqueeze(2).to_broadcast([128, 4, 8]), ALU.add, R, R)
                kb.rmax(l1, lm, R, R)
                kb.ts('dve', m1, lm, l1, ALU.is_equal, R, R)
                kb.stt('dve', lm, m1, -BIG, lm, ALU.mult, ALU.add, R, R)
                kb.rmax(l2, lm, R, R)
                kb.ts('dve', lm, lm, l2, ALU.is_equal, R, R)
                kb.ts('dve', nl1, l1, -1.0, ALU.mult, R, R)
                kb.act(e2, l2, AF.Exp, R, R, bias=nl1, scale=1.0)
                kb.ts('dve', w1g, e2, 1.0, ALU.add, R, R)
                kb.recip(w1g, w1g, R, R)
                kb.tt('dve', w1g, w1g, sg, ALU.mult, R, R)
                kb.tt('dve', w2g, w1g, e2, ALU.mult, R, R)
                kb.ts('dve', Gt[:, t, :], m1, w1g, ALU.mult, R, ['Gt'])
                kb.stt('dve', Gt[:, t, :], lm, w2g, Gt[:, t, :], ALU.mult, ALU.add, R + ['Gt'], ['Gt'])
            kb.end_phase(upto == 1)
            if upto == 1:
                return nc

        for half in range(2):
            t0 = half * 5
            with ExitStack() as hs:
                yacc = hs.enter_context(nc.sbuf_tensor("yacc%d" % half, [128, 5, D], F32))
                yn = 'yacc'
                kb.memset('pool', yacc[:], 0.0, [yn])
                with kb.phase():
                    h2T = kb.sb("h2T", [128, 16, 640], BF16)
                    for i in range(5):
                        kb.dma(h2T[:, :, i * 128:(i + 1) * 128], h2s[t0 + i], ['h2s%d' % (t0 + i)], ['h2T'])
                    w1b = [kb.sb("w1b%d" % i, [128, 16, 512], BF16) for i in range(2)]
                    w3b = [kb.sb("w3b%d" % i, [128, 16, 512], BF16) for i in range(2)]
                    w2b = [kb.sb("w2b%d" % i, [128, 4, D], BF16) for i in range(2)]
                    actb = kb.sb("actb", [128, 4, 640], BF16)
                    sil = [kb.sb("sil%d" % i, [128, 512], F32) for i in range(2)]
                    sil_rr = 0
                    for e_ in range(32):
                        bi = e_ % 2
                        kb.dma(w1b[bi][:], w1[e_].rearrange("(k p) f -> p k f", p=128), (), ['w1b%d' % bi], q='pool')
                        kb.dma(w3b[bi][:], w3[e_].rearrange("(k p) f -> p k f", p=128), (), ['w3b%d' % bi], q='pool')
                        kb.dma(w2b[bi][:], w2[e_].rearrange("(c p) n -> p c n", p=128), (), ['w2b%d' % bi], q='pool')
                        for c in range(4):
                            for (c0, n) in ((0, 512), (512, 128)):
                                p1, p1n = kb.pbank((0, 1))
                                p3, p3n = kb.pbank((2, 3))
                                for k in range(16):
                                    kb.mm(p1[:, :n], w1b[bi][:, k, c * 128:(c + 1) * 128], h2T[:, k, c0:c0 + n], k == 0, k == 15,
                                          ['w1b%d' % bi, 'h2T'], p1n)
                                for k in range(16):
                                    kb.mm(p3[:, :n], w3b[bi][:, k, c * 128:(c + 1) * 128], h2T[:, k, c0:c0 + n], k == 0, k == 15,
                                          ['w3b%d' % bi, 'h2T'], p3n)
                                sl = sil[sil_rr % 2]
                                sln = 'sil%d' % (sil_rr % 2)
                                sil_rr += 1
                                kb.act(sl[:, :n], p1[:, :n], AF.Silu, p1n, [sln])
                                kb.tt('dve', actb[:, c, c0:c0 + n], sl[:, :n], p3[:, :n], ALU.mult, [sln] + p3n, ['actb'])
                        for i in range(5):
                            for cb in range(4):
                                py, pyn = kb.pbank((4, 5))
                                for c in range(4):
                                    kb.mm(py[:, :], actb[:, c, i * 128:(i + 1) * 128], w2b[bi][:, c, cb * 512:(cb + 1) * 512],
                                          c == 0, c == 3, ['actb', 'w2b%d' % bi], pyn)
                                kb.stt('dve', yacc[:, i, cb * 512:(cb + 1) * 512], py[:, :], Gt[:, t0 + i, e_:e_ + 1],
                                       yacc[:, i, cb * 512:(cb + 1) * 512], ALU.mult, ALU.add, pyn + ['Gt', yn], [yn])
                    kb.end_phase(False)
                with kb.phase():
                    lnb = [kb.sb("ln2b%d" % i, [128, D], F32) for i in range(2)]
                    for i in range(2):
                        kb.dma(lnb[i][:], lnv[2 + i, :].partition_broadcast(128), (), ['ln2b%d' % i])
                    G2 = [kb.sb("G2_%d" % r, [128, D], F32) for r in range(2)]
                    for r in range(2):
                        if half == 0 or r == 0:
                            make_bc(G2[r], 48, r, 'G2_%d' % r)
                    x1l = [kb.sb("x1l%d" % i, [128, D], F32) for i in range(2)]
                    ot = [kb.sb("ot%d" % i, [128, D], F32) for i in range(2)]
                    stats = kb.sb("stats2", [128, 4, 6], F32)
                    mv = kb.sb("mv2", [128, 4], F32)
                    for i in range(5):
                        t = t0 + i
                        r = 1 if t < 2 else 0
                        x_ = x1l[i % 2]
                        xn_ = 'x1l%d' % (i % 2)
                        o_ = ot[i % 2]
                        on_ = 'ot%d' % (i % 2)
                        kb.dma(x_[:], x1s[t * 128:(t + 1) * 128, :], ['x1s%d' % t], [xn_])
                        kb.tt('dve', o_[:], yacc[:, i, :], G2[r][:], ALU.mult, [yn, 'G2_%d' % r], [on_])
                        kb.stt('dve', o_[:], x_[:], ALPHA, o_[:], ALU.mult, ALU.add, [xn_, on_], [on_])
                        for c in range(4):
                            P.op('dve', lambda e, c=c, o_=o_: e.bn_stats(out=stats[:, c, :], in_=o_[:, c * 512:(c + 1) * 512]),
                                 [on_], ['stats2'])
                        P.op('dve', lambda e: e.bn_aggr(out=mv[:, 0:2], in_=stats[:]), ['stats2'], ['mv2'])
                        kb.rsqrt(mv[:, 1:2], mv[:, 1:2], 1.0, ['mv2'], ['mv2'])
                        kb.ts('dve', mv[:, 2:3], mv[:, 0:1], mv[:, 1:2], ALU.mult, ['mv2'], ['mv2'], s2=-1.0, op1=ALU.mult)
                        kb.act(o_[:], o_[:], AF.Identity, [on_, 'mv2'], [on_], bias=mv[:, 2:3], scale=mv[:, 1:2])
                        kb.tt('dve', o_[:], o_[:], lnb[0][:], ALU.mult, [on_, 'ln2b0'], [on_])
                        kb.tt('pool', o_[:], o_[:], lnb[1][:], ALU.add, [on_, 'ln2b1'], [on_])
                        kb.dma(outp[t * 128:(t + 1) * 128, :], o_[:], [on_], ['outp%d' % t])
                    kb.end_phase(half == 1)
    return nc


def prep_B(inp, l, b, q, mixfull, x_cur, ctx_cur):
    f = lambda a: np.ascontiguousarray(a, dtype=np.float32)
    cvec = np.stack([inp["c"][b], inp["c_ctx"]], 0)
    cT = cvec.reshape(2, 16, 128).transpose(2, 1, 0).reshape(128, 32)
    badaT = inp["b_ada"][l][4096:].reshape(64, 128).T
    lat = slice(L + q * 1024, L + (q + 1) * 1024)
    return {
        "mixq": f(np.concatenate([mixfull[b][:, :L], mixfull[b][:, lat]], 1)),
        "xin": f(np.concatenate([ctx_cur[b], x_cur[b][q * 1024:(q + 1) * 1024]], 0)),
        "cT": f(cT), "wada": f(inp["w_ada"][l][:, 4096:]), "badaT": f(badaT),
        "wout": f(inp["w_out"][l]),
        "lnv": f(np.stack([inp["ln1_g"][l], inp["ln1_b"][l], inp["ln2_g"][l], inp["ln2_b"][l]], 0)),
        "wr": f(np.concatenate([inp["router_group"][l], inp["router_expert"][l]], 1)),
        "w1": f(inp["moe_w1"][l]), "w3": f(inp["moe_w3"][l]), "w2": f(inp["moe_w2"][l]),
    }


_CACHE = {}


def kernel(**inputs):
    inp = {k: np.asarray(v) for k, v in inputs.items()}
    if "A" not in _CACHE:
        _CACHE["A"] = build_A()
        _CACHE["B"] = build_B()
        _CACHE["tabs"] = _rope_tables()
    tabs = _CACHE["tabs"]
    x_cur = inp["x"].astype(np.float32)
    ctx_cur = inp["ctx"].astype(np.float32)
    cores = list(range(8))
    for l in range(2):
        mapsA = [prep_A(inp, l, i // 4, i % 4, x_cur, ctx_cur, tabs) for i in cores]
        resA = run_bass_kernel_spmd(_CACHE["A"], mapsA, core_ids=cores)
        mixfull = []
        for b in range(2):
            m = np.empty((D, NTOK), np.float32)
            for h in range(4):
                r = resA.results[b * 4 + h]["mixT"]
                for mi in range(4):
                    m[mi * 512 + h * 128: mi * 512 + (h + 1) * 128, :] = r[mi * 128:(mi + 1) * 128, :]
            mixfull.append(m)
        del resA, mapsA
        mapsB = [prep_B(inp, l, i // 4, i % 4, mixfull, x_cur, ctx_cur) for i in cores]
        resB = run_bass_kernel_spmd(_CACHE["B"], mapsB, core_ids=cores)
        x_new = np.empty_like(x_cur)
        ctx_new = np.empty_like(ctx_cur)
        for b in range(2):
            for q in range(4):
                o = resB.results[b * 4 + q]["outp"]
                x_new[b, q * 1024:(q + 1) * 1024] = o[L:]
                if q == 0:
                    ctx_new[b] = o[:L]
        del resB, mapsB
        x_cur, ctx_cur = x_new, ctx_new
    return x_cur
```
